# Optimizing a Trainium2 kernel written in Bass

```python
import math
import jax, jax.numpy as jnp
from jax import lax
import numpy as np

D_MODEL = 1024
BATCH = 8
SEQ = 2048
DEPTH = 1

GRID_W = 64
Q_BLOCK = 128
HEAD_DIM = 64
N_DIFF_HEADS = 4
DIFF_V_DIM = 2 * HEAD_DIM
DIFF_WIDTH = N_DIFF_HEADS * DIFF_V_DIM
N_GQA_HEADS = 8
N_GQA_KV = 2
GQA_REP = N_GQA_HEADS // N_GQA_KV
GQA_WIDTH = N_GQA_HEADS * HEAD_DIM
MIX_WIDTH = DIFF_WIDTH + GQA_WIDTH
DQ_COLS = N_DIFF_HEADS * 2 * HEAD_DIM
DK_COLS = N_DIFF_HEADS * 2 * HEAD_DIM
DV_COLS = DIFF_WIDTH
GQ_COLS = N_GQA_HEADS * HEAD_DIM
GK_COLS = N_GQA_KV * HEAD_DIM
GV_COLS = N_GQA_KV * HEAD_DIM
IN_COLS = DQ_COLS + DK_COLS + DV_COLS + GQ_COLS + GK_COLS + GV_COLS
SPLIT_POINTS = [DQ_COLS, DQ_COLS + DK_COLS, DQ_COLS + DK_COLS + DV_COLS,
                DQ_COLS + DK_COLS + DV_COLS + GQ_COLS,
                DQ_COLS + DK_COLS + DV_COLS + GQ_COLS + GK_COLS]
ROPE_THETA = 10000.0
ROPE_AXIS_DIM = HEAD_DIM // 2
N_BUCKETS = 32
MAX_DISTANCE = 128
N_GROUPS = 4
EXPERTS_PER_GROUP = 8
N_EXPERTS = N_GROUPS * EXPERTS_PER_GROUP
TOP_K_INNER = 2
D_EXPERT = D_MODEL // 2
LN_EPS = 1e-5
RMS_EPS = 1e-6
DEEPNORM_ALPHA = (2 * DEPTH) ** 0.25
DEEPNORM_BETA = (8 * DEPTH) ** -0.25

kernel_name = "hybrid_diffattn_axialgqa_hiermoe_encoder"


def layer_norm(x):
    xf = x.astype(jnp.float32)
    mu = jnp.mean(xf, -1, keepdims=True)
    var = jnp.mean(jnp.square(xf - mu), -1, keepdims=True)
    return ((xf - mu) * lax.rsqrt(var + LN_EPS)).astype(x.dtype)


def layer_norm_affine(x, g, b):
    xf = x.astype(jnp.float32)
    mu = jnp.mean(xf, -1, keepdims=True)
    var = jnp.mean(jnp.square(xf - mu), -1, keepdims=True)
    y = (xf - mu) * lax.rsqrt(var + LN_EPS) * g.astype(jnp.float32) + b.astype(jnp.float32)
    return y.astype(x.dtype)


def rms_norm(x, g):
    xf = x.astype(jnp.float32)
    y = xf * lax.rsqrt(jnp.mean(jnp.square(xf), -1, keepdims=True) + RMS_EPS) * g.astype(jnp.float32)
    return y.astype(x.dtype)


def t5_bucket(rel):
    half = N_BUCKETS // 2
    max_exact = half // 2
    ret = jnp.where(rel > 0, half, 0)
    n = jnp.abs(rel)
    nf = jnp.maximum(n, 1).astype(jnp.float32)
    large = max_exact + (jnp.log(nf / max_exact) / math.log(MAX_DISTANCE / max_exact)
                         * (half - max_exact)).astype(jnp.int32)
    large = jnp.minimum(large, half - 1)
    return ret + jnp.where(n < max_exact, n, large)


def axial_rope_tables(seq_len):
    n_rows = seq_len // GRID_W
    rr, cc = jnp.meshgrid(jnp.arange(n_rows), jnp.arange(GRID_W), indexing="ij")
    row = rr.reshape(-1).astype(jnp.float32)
    col = cc.reshape(-1).astype(jnp.float32)
    freqs = ROPE_THETA ** (-jnp.arange(0, ROPE_AXIS_DIM, 2, dtype=jnp.float32) / ROPE_AXIS_DIM)
    ang = jnp.concatenate([row[:, None] * freqs, col[:, None] * freqs], -1)
    return jnp.cos(ang), jnp.sin(ang)


def apply_rope(x, cos, sin):
    xf = x.astype(jnp.float32).reshape(x.shape[:-1] + (HEAD_DIM // 2, 2))
    x1, x2 = xf[..., 0], xf[..., 1]
    c = cos[None, :, None, :]
    s = sin[None, :, None, :]
    out = jnp.stack([x1 * c - x2 * s, x1 * s + x2 * c], -1).reshape(x.shape)
    return out.astype(x.dtype)


def hybrid_mixer(h, w_in, w_out, lam_q1, lam_k1, lam_q2, lam_k2, diff_subln_g,
                 q_norm_g, k_norm_g, rel_bias, lambda_init):
    B, S, _ = h.shape
    n_blocks = S // Q_BLOCK
    proj = h @ w_in
    dq, dk, dv, gq, gk, gv = jnp.split(proj, SPLIT_POINTS, axis=-1)
    dq = dq.reshape(B, S, N_DIFF_HEADS, 2, HEAD_DIM)
    dk = dk.reshape(B, S, N_DIFF_HEADS, 2, HEAD_DIM)
    dv = dv.reshape(B, S, N_DIFF_HEADS, DIFF_V_DIM)
    gq = gq.reshape(B, S, N_GQA_HEADS, HEAD_DIM)
    gk = gk.reshape(B, S, N_GQA_KV, HEAD_DIM)
    gv = gv.reshape(B, S, N_GQA_KV, HEAD_DIM)

    cos, sin = axial_rope_tables(S)
    gq = apply_rope(rms_norm(gq, q_norm_g), cos, sin)
    gk = apply_rope(rms_norm(gk, k_norm_g), cos, sin)

    f32 = jnp.float32
    lam = (jnp.exp(jnp.sum(lam_q1.astype(f32) * lam_k1.astype(f32)))
           - jnp.exp(jnp.sum(lam_q2.astype(f32) * lam_k2.astype(f32))) + lambda_init)

    scale = HEAD_DIM ** -0.5
    dq_b = (dq * scale).reshape(B, n_blocks, Q_BLOCK, N_DIFF_HEADS, 2, HEAD_DIM).transpose(1, 0, 2, 3, 4, 5)
    gq_b = (gq * scale).reshape(B, n_blocks, Q_BLOCK, N_GQA_KV, GQA_REP, HEAD_DIM).transpose(1, 0, 2, 3, 4, 5)
    kpos = jnp.arange(S, dtype=jnp.int32)

    def block(args):
        blk, dqb, gqb = args
        qpos = blk * Q_BLOCK + jnp.arange(Q_BLOCK, dtype=jnp.int32)
        bucket = t5_bucket(kpos[None, :] - qpos[:, None])
        bias = rel_bias[bucket].transpose(2, 0, 1).astype(f32)
        sd = jnp.einsum("bqhcd,bkhcd->bhcqk", dqb, dk).astype(f32) + bias[None, :, None]
        pd = jax.nn.softmax(sd, axis=-1)
        ad = pd[:, :, 0] - lam * pd[:, :, 1]
        od = jnp.einsum("bhqk,bkhe->bqhe", ad.astype(dv.dtype), dv)
        sg = jnp.einsum("bqgrd,bkgd->bgrqk", gqb, gk).astype(f32)
        pg = jax.nn.softmax(sg, axis=-1)
        og = jnp.einsum("bgrqk,bkgd->bqgrd", pg.astype(gv.dtype), gv)
        return od, og

    od, og = lax.map(block, (jnp.arange(n_blocks, dtype=jnp.int32), dq_b, gq_b))
    od = od.transpose(1, 0, 2, 3, 4).reshape(B, S, N_DIFF_HEADS, DIFF_V_DIM)
    od = rms_norm(od, diff_subln_g) * (1.0 - lambda_init)
    og = og.transpose(1, 0, 2, 3, 4, 5).reshape(B, S, GQA_WIDTH)
    mixed = jnp.concatenate([od.reshape(B, S, DIFF_WIDTH), og], axis=-1)
    return mixed @ w_out


def hier_moe(h, w_rg, b_rg, w_re, b_re, w_gate, w_up, w_down):
    B, S, D = h.shape
    t = h.reshape(B * S, D)
    g_logits = (t @ w_rg + b_rg).astype(jnp.float32)
    g_prob = jax.nn.softmax(g_logits, axis=-1)
    g_sel = jnp.argmax(g_logits, axis=-1)
    p_g = jnp.take_along_axis(g_prob, g_sel[:, None], axis=1)[:, 0]
    e_all = (jnp.einsum("td,gde->tge", t, w_re) + b_re).astype(jnp.float32)
    e_logits = jnp.take_along_axis(e_all, g_sel[:, None, None], axis=1)[:, 0]
    top_v, top_i = lax.top_k(e_logits, TOP_K_INNER)
    w2 = jax.nn.softmax(top_v, axis=-1) * p_g[:, None]
    expert_id = g_sel[:, None] * EXPERTS_PER_GROUP + top_i
    combine = jnp.sum(jax.nn.one_hot(expert_id, N_EXPERTS, dtype=jnp.float32) * w2[..., None], axis=1)

    def expert_step(acc, params):
        wg, wu, wd, cw = params
        hid = jax.nn.silu(t @ wg) * (t @ wu)
        return acc + cw[:, None] * (hid @ wd), None

    out, _ = lax.scan(expert_step, jnp.zeros_like(t),
                      (w_gate, w_up, w_down, combine.T.astype(t.dtype)))
    return out.reshape(B, S, D)


def setup_inputs(seed: int = 0) -> dict:
    key = jax.random.key(seed)
    ks = jax.random.split(key, 25)
    L, D = DEPTH, D_MODEL

    def nrm(k, shape, s):
        return jax.random.normal(k, shape, jnp.float32) * s

    col_scale = np.concatenate([
        np.ones(DQ_COLS + DK_COLS), np.full(DV_COLS, DEEPNORM_BETA),
        np.ones(GQ_COLS + GK_COLS), np.full(GV_COLS, DEEPNORM_BETA)]).astype(np.float32)
    return {
        "x": nrm(ks[0], (BATCH, SEQ, D), 1.0),
        "c": nrm(ks[1], (BATCH, D), 1.0),
        "w_ada": nrm(ks[2], (L, D, 6 * D), 0.1 * D ** -0.5),
        "b_ada": nrm(ks[3], (L, 6 * D), 0.01),
        "w_in": nrm(ks[4], (L, D, IN_COLS), D ** -0.5) * jnp.asarray(col_scale),
        "lambda_q1": nrm(ks[5], (L, HEAD_DIM), 0.1),
        "lambda_k1": nrm(ks[6], (L, HEAD_DIM), 0.1),
        "lambda_q2": nrm(ks[7], (L, HEAD_DIM), 0.1),
        "lambda_k2": nrm(ks[8], (L, HEAD_DIM), 0.1),
        "diff_subln_g": 1.0 + nrm(ks[9], (L, DIFF_V_DIM), 0.01),
        "q_norm_g": 1.0 + nrm(ks[10], (L, HEAD_DIM), 0.01),
        "k_norm_g": 1.0 + nrm(ks[11], (L, HEAD_DIM), 0.01),
        "rel_bias": nrm(ks[12], (N_BUCKETS, N_DIFF_HEADS), 0.5),
        "w_out": nrm(ks[13], (L, MIX_WIDTH, D), DEEPNORM_BETA * MIX_WIDTH ** -0.5),
        "ln1_g": 1.0 + nrm(ks[14], (L, D), 0.01),
        "ln1_b": nrm(ks[15], (L, D), 0.01),
        "w_router_group": nrm(ks[16], (L, D, N_GROUPS), D ** -0.5),
        "b_router_group": nrm(ks[17], (L, N_GROUPS), 0.01),
        "w_router_expert": nrm(ks[18], (L, N_GROUPS, D, EXPERTS_PER_GROUP), D ** -0.5),
        "b_router_expert": nrm(ks[19], (L, N_GROUPS, EXPERTS_PER_GROUP), 0.01),
        "w_gate": nrm(ks[20], (L, N_EXPERTS, D, D_EXPERT), D ** -0.5),
        "w_up": nrm(ks[21], (L, N_EXPERTS, D, D_EXPERT), D ** -0.5),
        "w_down": nrm(ks[22], (L, N_EXPERTS, D_EXPERT, D), DEEPNORM_BETA * D_EXPERT ** -0.5),
        "ln2_g": 1.0 + nrm(ks[23], (L, D), 0.01),
        "ln2_b": nrm(ks[24], (L, D), 0.01),
    }


def reference(x, c, w_ada, b_ada, w_in, lambda_q1, lambda_k1, lambda_q2, lambda_k2,
              diff_subln_g, q_norm_g, k_norm_g, rel_bias, w_out, ln1_g, ln1_b,
              w_router_group, b_router_group, w_router_expert, b_router_expert,
              w_gate, w_up, w_down, ln2_g, ln2_b):
    for l in range(DEPTH):
        lambda_init = 0.8 - 0.6 * math.exp(-0.3 * l)
        mod = jax.nn.silu(c) @ w_ada[l] + b_ada[l]
        sh1, sc1, g1, sh2, sc2, g2 = [m[:, None, :] for m in jnp.split(mod, 6, axis=-1)]
        h = layer_norm(x) * (1 + sc1) + sh1
        mix = hybrid_mixer(h, w_in[l], w_out[l], lambda_q1[l], lambda_k1[l], lambda_q2[l],
                           lambda_k2[l], diff_subln_g[l], q_norm_g[l], k_norm_g[l],
                           rel_bias, lambda_init)
        x = layer_norm_affine(DEEPNORM_ALPHA * x + (1 + g1) * mix, ln1_g[l], ln1_b[l])
        h = layer_norm(x) * (1 + sc2) + sh2
        ffn = hier_moe(h, w_router_group[l], b_router_group[l], w_router_expert[l],
                       b_router_expert[l], w_gate[l], w_up[l], w_down[l])
        x = layer_norm_affine(DEEPNORM_ALPHA * x + (1 + g2) * ffn, ln2_g[l], ln2_b[l])
    return x
```

```python
import math
from contextlib import ExitStack

import numpy as np
import concourse.bass as bass
import concourse.mybir as mybir
from concourse.bass_utils import run_bass_kernel_spmd

F32 = mybir.dt.float32
BF16 = mybir.dt.bfloat16
I32 = mybir.dt.int32
AF = mybir.ActivationFunctionType
ALU = mybir.AluOpType
AX = mybir.AxisListType

ENGS = ("pe", "act", "dve", "pool", "sp")
N_DSEM = 8

S = 2048
D = 1024
NT = 16
NE = 32
ALPHA = 2.0 ** 0.25
LAMBDA_INIT = 0.2
LN_EPS = 1e-5
RMS_EPS = 1e-6
N_MOE_EXPERTS = NE
DBG = {}


class Prog:
    def __init__(self, nc):
        self.nc = nc
        self.q = {e: [] for e in ENGS}
        self.seq = {e: 0 for e in ENGS}
        self.seen = {e: {} for e in ENGS}
        self.res = {}
        self.dma_rot = {"sp": 0, "pool": 0}
        self.dma_cnt = {}
        self.skip_ops = None

    def _need(self, eng, tok, waits):
        if tok is None:
            return
        key, val = tok
        if key == eng and eng == "pe":
            return
        cur = self.seen[eng].get(key, 0)
        if val > cur:
            self.seen[eng][key] = val
            waits[key] = max(waits.get(key, 0), val)

    def _deps(self, eng, reads, writes, waits):
        for r in reads:
            st = self.res.get(r)
            if st is not None:
                self._need(eng, st["w"], waits)
                if isinstance(r, tuple) and r[0] == "ps":
                    for t in st["r"]:
                        if t[0] != eng:
                            self._need(eng, t, waits)
        for w in writes:
            st = self.res.get(w)
            if st is not None:
                self._need(eng, st["w"], waits)
                for t in st["r"]:
                    self._need(eng, t, waits)

    def _record(self, tok, reads, writes):
        for r in reads:
            st = self.res.setdefault(r, {"w": None, "r": []})
            st["r"].append(tok)
            if len(st["r"]) > 48:
                best = {}
                for k, v in st["r"]:
                    best[k] = max(best.get(k, 0), v)
                st["r"] = list(best.items())
        for w in writes:
            self.res[w] = {"w": tok, "r": []}

    def alias(self, new, olds):
        toks = []
        for o in olds:
            st = self.res.get(o)
            if st is not None:
                if st["w"] is not None:
                    toks.append(st["w"])
                toks.extend(st["r"])
        self.res[new] = {"w": None, "r": toks}

    def op(self, eng, fn, reads=(), writes=(), inc=True):
        waits = {}
        self._deps(eng, reads, writes, waits)
        if inc:
            self.seq[eng] += 1
            tok = (eng, self.seq[eng])
        else:
            tok = (eng, self.seq[eng] + 1)
        self.q[eng].append(("op", fn, waits, inc))
        self._record(tok, reads, writes)
        return tok

    def dma(self, queue, fn, reads=(), writes=()):
        waits = {}
        self._deps(queue, reads, writes, waits)
        i = self.dma_rot[queue]
        self.dma_rot[queue] = (i + 1) % N_DSEM
        key = ("d", queue, i)
        prev = self.dma_cnt.get(key, 0)
        if prev:
            self._need(queue, (key, prev), waits)
        self.dma_cnt[key] = prev + 16
        tok = (key, prev + 16)
        self.q[queue].append(("dma", fn, waits, key))
        self._record(tok, reads, writes)
        return tok

    def vload(self, name, ap, reads, min_val=0, max_val=64):
        for eng in ("pe", "act", "dve"):
            waits = {}
            self._deps(eng, reads, [], waits)
            self.q[eng].append(("vload", (name, ap, min_val, max_val), waits, None))

    def begin_if(self, cond):
        if not hasattr(self, "_if_stack"):
            self._if_stack = []
        snap = {e: dict(self.seen[e]) for e in ("pe", "act", "dve")}
        self._if_stack.append(({e: self.seq[e] for e in ("pe", "act", "dve")}, snap))
        for eng in ("pe", "act", "dve"):
            self.q[eng].append(("if", cond, {}, None))

    def end_if(self):
        st, snap = self._if_stack.pop()
        if not hasattr(self, "_skip_rot"):
            self._skip_rot = {e: 0 for e in ("pe", "act", "dve")}
            self._skip_last = {e: [0] * 16 for e in ("pe", "act", "dve")}
        for eng in ("pe", "act", "dve"):
            k = self.seq[eng] - st[eng]
            extra = None
            if k > 0:
                slot = self._skip_rot[eng] % 16
                self._skip_rot[eng] += 1
                extra = (slot, min(self._skip_last[eng][slot], st[eng]))
                self._skip_last[eng][slot] = self.seq[eng]
            self.q[eng].append(("else", k, {}, extra))
        for e in ("pe", "act", "dve"):
            self.seen[e] = snap[e]

    def scope(self, name):
        for e in ENGS:
            self.q[e].append(("scope", name, {}, None))

    def barrier(self):
        toks = [(e, self.seq[e]) for e in ("pe", "act", "dve", "pool") if self.seq[e] > 0]
        toks += [(k, v) for k, v in self.dma_cnt.items()]
        for e in ENGS:
            self.wait_all(e, toks)

    def wait_all(self, eng, toks):
        waits = {}
        for t in toks:
            self._need(eng, t, waits)
        self.q[eng].append(("wait", None, waits, None))

    def emit(self):
        nc = self.nc
        with ExitStack() as es:
            sems = {}
            for e in ENGS:
                sems[e] = es.enter_context(nc.semaphore("s_" + e))
            for qn in ("sp", "pool"):
                for i in range(N_DSEM):
                    sems[("d", qn, i)] = es.enter_context(nc.semaphore("d_%s%d" % (qn, i)))
            block = es.enter_context(nc.Block())

            def run(ename):
                def body(eng):
                    vals = {}
                    ctx = []
                    cur_scope = [None]
                    for kind, fn, waits, extra in self.q[ename]:
                        if kind == "scope":
                            if not DBG.get("scopes"):
                                continue
                            if cur_scope[0] is not None:
                                nc.leave_named_scope(cur_scope[0][0], cur_scope[0][1], False)
                            sid, _ = nc.enter_named_scope(fn, False)
                            cur_scope[0] = (fn, sid)
                            continue
                        for k, v in waits.items():
                            eng.wait_ge(sems[k], v)
                        if kind == "vload":
                            name, ap, mn, mx = fn
                            vals[name] = eng.value_load(ap)
                        elif kind == "if":
                            c = eng.If(fn(vals))
                            c.__enter__()
                            ctx.append(c)
                        elif kind == "else":
                            c = ctx.pop()
                            c.__exit__(None, None, None)
                            if fn > 0:
                                c2 = eng.Else()
                                c2.__enter__()
                                if DBG.get("drain_skip") or self.skip_ops is None:
                                    eng.drain()
                                    eng.sem_inc(sems[ename], fn)
                                else:
                                    slot, prev_tok = extra
                                    if prev_tok:
                                        eng.wait_ge(sems[ename], prev_tok)
                                    self.skip_ops[ename](eng, slot).then_inc(sems[ename], fn)
                                c2.__exit__(None, None, None)
                        elif kind == "op":
                            ins = fn(eng)
                            if extra:
                                ins.then_inc(sems[ename], 1)
                        elif kind == "dma":
                            ins = fn(eng)
                            ins.then_inc(sems[extra], 16)
                    if cur_scope[0] is not None:
                        nc.leave_named_scope(cur_scope[0][0], cur_scope[0][1], False)
                return body

            block.tensor(run("pe"))
            block.scalar(run("act"))
            block.vector(run("dve"))
            block.gpsimd(run("pool"))
            block.sync(run("sp"))


class _Stop(Exception):
    pass


def build_program(dbg=None, stop=None):
    nc = bass.Bass("TRN2", target_bir_lowering=False)

    def din(name, shape):
        return nc.dram_tensor(name, list(shape), F32, kind="ExternalInput").ap()

    x_d = din("x", [S, D])
    c_d = din("c", [D])
    w_ada_d = din("w_ada", [D, 6 * D])
    b_ada_d = din("b_ada", [6 * D])
    w_in_d = din("w_in", [D, 2304])
    lam_d = din("lam", [4, 64])
    subg_d = din("subg", [128])
    qg_d = din("qg", [64])
    kg_d = din("kg", [64])
    tb_d = din("tbias", [128, 4 * 384])
    cb_d = din("cbias", [128, 8])
    rc_d = din("ropec", [128, NT * 64])
    rs_d = din("ropes", [128, NT * 64])
    w_out_d = din("w_out", [D, D])
    ln1g_d = din("ln1_g", [D])
    ln1b_d = din("ln1_b", [D])
    wr_d = din("w_rt", [D, 36])
    br_d = din("b_rt", [36])
    wg_d = din("w_gate", [NE, D, 512])
    wu_d = din("w_up", [NE, D, 512])
    wd_d = din("w_down", [NE, 512, D])
    ln2g_d = din("ln2_g", [D])
    ln2b_d = din("ln2_b", [D])
    y_d = nc.dram_tensor("y", [S, D], F32, kind="ExternalOutput").ap()
    Hn_d = nc.dram_tensor("hn_scr", [20 * 128, D], BF16, kind="Internal").ap()
    Xs_d = nc.dram_tensor("xs_scr", [20 * 128, D], F32, kind="Internal").ap()
    Cs_d = nc.dram_tensor("cs_scr", [20 * 128, 48], F32, kind="Internal").ap()
    Ys_d = nc.dram_tensor("ys_scr", [20 * 128, D], F32, kind="Internal").ap()
    dbg_d = None
    if dbg is not None:
        dbg_d = nc.dram_tensor("dbg", list(dbg), F32, kind="ExternalOutput").ap()

    es = ExitStack()
    ARENA_BYTES = 204 * 1024
    arena = es.enter_context(nc.sbuf_tensor("arena", [128, ARENA_BYTES // 4], F32))
    psb = [es.enter_context(nc.psum_tensor("ps%d" % i, [128, 512], F32)) for i in range(8)]

    def view(off, shape, dt):
        n = 1
        for s_ in shape[1:]:
            n *= s_
        nb = n * (2 if dt == BF16 else 4)
        assert off % 4 == 0 and off + nb <= ARENA_BYTES, (off, nb)
        ap = arena[:, off // 4:(off + nb) // 4]
        if dt == BF16:
            ap = ap.bitcast(BF16)
        if len(shape) == 3:
            ap = ap.rearrange("p (a b) -> p a b", b=shape[2])
        elif len(shape) == 4:
            ap = ap.rearrange("p (a b c) -> p a b c", b=shape[2], c=shape[3])
        return ap

    class Alloc:
        def __init__(self, base, limit):
            self.off = base
            self.limit = limit

        def __call__(self, shape, dt):
            n = 1
            for s_ in shape[1:]:
                n *= s_
            nb = n * (2 if dt == BF16 else 4)
            nb = (nb + 63) // 64 * 64
            v = view(self.off, shape, dt)
            self.off += nb
            assert self.off <= self.limit, (self.off, self.limit)
            return v

    KB = 1024
    ca = Alloc(0, 30 * KB)
    ca.limit = 30 * KB
    identf = ca([128, 128], F32)
    ident = ca([128, 128], BF16)
    ones_bf = ca([128, 128], BF16)
    swapM = ca([128, 128], F32)
    Tb = ca([128, 4, 384], F32)
    ropeC = ca([128, NT, 64], F32)
    ropeS = ca([128, NT, 64], F32)
    gB = ca([128, 10, 64], F32)
    g1B = ca([128, D], F32)
    g2B = ca([128, D], F32)
    modT = ca([128, 4, 8], F32)
    cbias = ca([128, 8], F32)
    nlam = ca([128, 1], F32)
    gsub = ca([128, 1], F32)
    epsL = ca([128, 1], F32)
    epsR = ca([128, 1], F32)
    cw = ca([128, NT, 32], F32)
    smallf = ca([128, 256], F32)
    rs2 = ca([128, NT], F32)
    nm2 = ca([128, NT], F32)
    posi = ca([128, NT], F32).bitcast(I32)
    tsi = ca([128, 8], F32).bitcast(I32)
    H_OFF = 30 * KB
    Q_OFF = 70 * KB
    S_OFF = 150 * KB
    hT = view(H_OFF, [128, 8, S], BF16)
    mixT = hT
    NSL = 20
    NSLOT = NSL * 128
    Xr = view(Q_OFF, [128, NSL, D], F32)
    hTs = view(H_OFF, [128, 8, NSLOT], BF16)

    P = Prog(nc)
    ps = [p[:] for p in psb]
    psbf = [p[:].bitcast(BF16) for p in psb]
    skp = ca([128, 16], F32)
    P.skip_ops = {
        "pe": lambda e, i: e.matmul(ps[7][0:1, 2 * i:2 * i + 2], lhsT=ident[0:1, 0:1], rhs=ident[0:1, 0:2], start=True, stop=True),
        "act": lambda e, i: e.activation(out=smallf[0:1, 2 * i:2 * i + 2], in_=skp[0:1, 0:2], func=AF.Silu),
        "dve": lambda e, i: e.tensor_copy(out=smallf[0:1, 32 + 2 * i:34 + 2 * i], in_=skp[0:1, 0:2]),
    }

    def chk(k):
        if stop is not None and stop == k:
            raise _Stop()

    try:
        sa = Alloc(S_OFF, ARENA_BYTES)
        P.op("pool", lambda e: e.memset(identf, 0.0), writes=["identf"])
        P.op("pool", lambda e: e.affine_select(out=identf, in_=identf, pattern=[[-1, 128]], compare_op=ALU.not_equal,
                                               fill=1.0, base=0, channel_multiplier=1), reads=["identf"], writes=["identf"])
        P.op("pool", lambda e: e.memset(swapM, 0.0), writes=["swapM"])
        P.op("pool", lambda e: e.affine_select(out=swapM, in_=swapM, pattern=[[-1, 128]], compare_op=ALU.not_equal,
                                               fill=1.0, base=64, channel_multiplier=1), reads=["swapM"], writes=["swapM"])
        P.op("pool", lambda e: e.affine_select(out=swapM, in_=swapM, pattern=[[-1, 128]], compare_op=ALU.not_equal,
                                               fill=1.0, base=-64, channel_multiplier=1), reads=["swapM"], writes=["swapM"])
        P.op("pool", lambda e: e.memset(ones_bf, 1.0), writes=["ones"])
        P.op("pool", lambda e: e.memset(skp, 0.0), writes=["skp"])
        P.op("pool", lambda e: e.memset(epsL, LN_EPS), writes=["eps"])
        P.op("pool", lambda e: e.memset(epsR, RMS_EPS), writes=["eps"])
        P.op("dve", lambda e: e.tensor_copy(out=ident, in_=identf), reads=["identf"], writes=["ident"])

        P.dma("sp", lambda e: e.dma_start(out=Tb.rearrange("p a b -> p (a b)"), in_=tb_d), writes=["Tb"])
        P.dma("sp", lambda e: e.dma_start(out=cbias, in_=cb_d), writes=["cbias"])
        P.dma("sp", lambda e: e.dma_start(out=ropeC.rearrange("p a b -> p (a b)"), in_=rc_d), writes=["rope"])
        P.dma("sp", lambda e: e.dma_start(out=ropeS.rearrange("p a b -> p (a b)"), in_=rs_d), writes=["rope"])
        lamv = smallf[:, 0:256].rearrange("p (a b) -> p a b", b=64)
        P.dma("sp", lambda e: e.dma_start(out=smallf[:, 0:256], in_=lam_d.rearrange("a b -> (a b)").partition_broadcast(128)),
              writes=["lamv"])
        sc_f = sa([128, 8], F32)
        sc_s = sa([128, 8], F32)
        scB = sa([128, 8, 128], BF16)
        qgk = sa([128, 2, 64], F32)
        lp = sa([128, 2, 64], F32)
        ls = sa([128, 2], F32)
        le = sa([128, 2], F32)
        P.dma("sp", lambda e: e.dma_start(out=sc_f, in_=c_d.rearrange("(c p) -> p c", p=128), allow_slow_non_contiguous=True),
              writes=["sc_f"])
        P.dma("sp", lambda e: e.dma_start(out=qgk[:, 0, :], in_=qg_d.partition_broadcast(128)), writes=["qgk0"])
        P.dma("sp", lambda e: e.dma_start(out=qgk[:, 1, :], in_=kg_d.partition_broadcast(128)), writes=["qgk1"])
        P.dma("sp", lambda e: e.dma_start(out=gsub, in_=subg_d.rearrange("(p o) -> p o", o=1)), writes=["gsub"])
        P.op("dve", lambda e: e.tensor_scalar(out=gB[:, 0:8, :], in0=qgk[:, 0:1, :].to_broadcast([128, 8, 64]), scalar1=0.125,
                                              scalar2=None, op0=ALU.mult), reads=["qgk0"], writes=["gBq"])
        P.op("dve", lambda e: e.tensor_copy(out=gB[:, 8:10, :], in_=qgk[:, 1:2, :].to_broadcast([128, 2, 64])),
             reads=["qgk1"], writes=["gBk"])
        P.op("dve", lambda e: e.tensor_scalar(out=gsub, in0=gsub, scalar1=1.0 - LAMBDA_INIT, scalar2=None, op0=ALU.mult),
             reads=["gsub"], writes=["gsub"])
        P.op("dve", lambda e: e.tensor_tensor(out=lp[:, 0, :], in0=lamv[:, 0, :], in1=lamv[:, 1, :], op=ALU.mult),
             reads=["lamv"], writes=["lp0"])
        P.op("dve", lambda e: e.tensor_tensor(out=lp[:, 1, :], in0=lamv[:, 2, :], in1=lamv[:, 3, :], op=ALU.mult),
             reads=["lamv"], writes=["lp1"])
        P.op("dve", lambda e: e.tensor_reduce(out=ls, in_=lp, axis=AX.X, op=ALU.add), reads=["lp0", "lp1"], writes=["ls"])
        P.op("act", lambda e: e.activation(out=le, in_=ls, func=AF.Exp), reads=["ls"], writes=["le"])
        P.op("dve", lambda e: e.tensor_scalar(out=nlam, in0=le[:, 1:2], scalar1=le[:, 0:1], scalar2=-LAMBDA_INIT,
                                              op0=ALU.subtract, op1=ALU.add), reads=["le"], writes=["nlam"])
        P.op("act", lambda e: e.activation(out=sc_s, in_=sc_f, func=AF.Silu), reads=["sc_f"], writes=["sc_s"])
        P.op("dve", lambda e: e.tensor_copy(out=scB, in_=sc_s.unsqueeze(2).to_broadcast([128, 8, 128])),
             reads=["sc_s"], writes=["scB"])

        chk(-1)
        P.scope("p0_adaln")
        wa = [sa([128, 8, 512], BF16) for _ in range(4)]
        ba = [sa([128, 512], F32) for _ in range(2)]
        modrow = [sa([128, 512], F32) for _ in range(2)]
        for j in range(12):
            b = j % 2
            wb = j % 4
            P.dma("pool", lambda e, j=j, wb=wb: e.dma_start(out=wa[wb], in_=w_ada_d[:, j * 512:(j + 1) * 512].rearrange("(c p) n -> p c n", p=128)),
                  writes=[("wa", wb)])
            P.dma("sp", lambda e, j=j, b=b: e.dma_start(out=ba[b], in_=b_ada_d[j * 512:(j + 1) * 512].partition_broadcast(128)),
                  writes=[("ba", b)])
            pb = j % 2
            for c in range(8):
                P.op("pe", lambda e, c=c, wb=wb, pb=pb: e.matmul(ps[pb], lhsT=scB[:, c, :], rhs=wa[wb][:, c, :], start=(c == 0), stop=(c == 7)),
                     reads=["scB", ("wa", wb)], writes=[("ps", pb)], inc=(c == 7))
            sect = j // 2
            half = j % 2
            if sect in (2, 5):
                dst = (g1B if sect == 2 else g2B)[:, half * 512:(half + 1) * 512]
                P.op("dve", lambda e, dst=dst, b=b, pb=pb: e.scalar_tensor_tensor(out=dst, in0=ps[pb], scalar=1.0, in1=ba[b],
                                                                                 op0=ALU.add, op1=ALU.add),
                     reads=[("ps", pb), ("ba", b)], writes=[("gB", sect, half)])
            else:
                addc = 1.0 if sect in (1, 4) else 0.0
                P.op("dve", lambda e, b=b, pb=pb, addc=addc: e.scalar_tensor_tensor(out=modrow[b], in0=ps[pb], scalar=addc, in1=ba[b],
                                                                                   op0=ALU.add, op1=ALU.add),
                     reads=[("ps", pb), ("ba", b)], writes=[("modrow", b)])
                mi = {0: 0, 1: 1, 3: 2, 4: 3}[sect]
                pc = 2 + (j % 2)
                for i in range(4):
                    P.op("pe", lambda e, i=i, b=b, pc=pc: e.matmul(ps[pc][:, i:i + 1], lhsT=modrow[b][:, i * 128:(i + 1) * 128],
                                                                   rhs=identf[:, 0:1], start=True, stop=True),
                         reads=[("modrow", b), "identf"], writes=[("ps", pc)], inc=(i == 3))
                P.op("dve", lambda e, mi=mi, half=half, pc=pc: e.tensor_copy(out=modT[:, mi, half * 4:(half + 1) * 4], in_=ps[pc][:, 0:4]),
                     reads=[("ps", pc)], writes=[("modT", mi, half)])
        MODT_ALL = [("modT", mi, h_) for mi in range(4) for h_ in range(2)]
        G1B = [("gB", 2, 0), ("gB", 2, 1)]
        G2B = [("gB", 5, 0), ("gB", 5, 1)]

        def ln_stats(src_ap, tag, scr):
            st, mv, lnv, rstd, nmr = scr
            for jj in range(2):
                P.op("dve", lambda e, jj=jj: e.bn_stats(out=st[:, jj, :], in_=src_ap[:, jj * 512:(jj + 1) * 512]),
                     reads=[tag], writes=[("st", id(st), jj)])
            P.op("dve", lambda e: e.bn_aggr(out=mv, in_=st.rearrange("p a b -> p (a b)")),
                 reads=[("st", id(st), 0), ("st", id(st), 1)], writes=[("mv", id(mv))])
            P.op("act", lambda e: e.activation(out=lnv, in_=mv[:, 1:2], func=AF.Ln, bias=epsL, scale=1.0),
                 reads=[("mv", id(mv)), "eps"], writes=[("lnv", id(lnv))])
            P.op("act", lambda e: e.activation(out=rstd, in_=lnv, func=AF.Exp, scale=-0.5),
                 reads=[("lnv", id(lnv))], writes=[("rstd", id(rstd))])
            P.op("dve", lambda e: e.scalar_tensor_tensor(out=nmr, in0=mv[:, 0:1], scalar=-1.0, in1=rstd, op0=ALU.mult, op1=ALU.mult),
                 reads=[("mv", id(mv)), ("rstd", id(rstd))], writes=[("nmr", id(nmr))])
            return [("rstd", id(rstd)), ("nmr", id(nmr))]

        def ln_scr(al):
            return (al([128, 2, 6], F32), al([128, 2], F32), al([128, 1], F32), al([128, 1], F32), al([128, 1], F32))

        def to_hT(xn_ap, xn_res, g4, sect_scale, sect_bias, psrot, hT=hT, hres="hT"):
            for c in range(8):
                pb = psrot[c % len(psrot)]
                for t in range(4):
                    P.op("pe", lambda e, c=c, t=t, pb=pb: e.transpose(out=psbf[pb][:, t * 128:(t + 1) * 128],
                                                                     in_=xn_ap[:, t, c * 128:(c + 1) * 128], identity=ident),
                         reads=[xn_res, "ident"], writes=[("ps", pb)], inc=(t == 3))
                eng = "act" if c % 2 == 0 else "dve"
                if eng == "act":
                    P.op("act", lambda e, c=c, pb=pb: e.activation(out=hT[:, c, g4 * 512:(g4 + 1) * 512], in_=psbf[pb][:, 0:512],
                                                                   func=AF.Identity, scale=modT[:, sect_scale, c:c + 1],
                                                                   bias=modT[:, sect_bias, c:c + 1]),
                         reads=[("ps", pb)] + MODT_ALL, writes=[(hres, g4)])
                else:
                    P.op("dve", lambda e, c=c, pb=pb: e.tensor_scalar(out=hT[:, c, g4 * 512:(g4 + 1) * 512], in0=psbf[pb][:, 0:512],
                                                                      scalar1=modT[:, sect_scale, c:c + 1],
                                                                      scalar2=modT[:, sect_bias, c:c + 1], op0=ALU.mult, op1=ALU.add),
                         reads=[("ps", pb)] + MODT_ALL, writes=[(hres, g4)])

        chk(0)
        P.scope("p1_ln")
        sa1 = Alloc(S_OFF + 51 * KB, ARENA_BYTES)
        xt = [view(Q_OFF + i * 16 * KB, [128, 4, D], F32) for i in range(2)]
        xn = [view(Q_OFF + 32 * KB + i * 8 * KB, [128, 4, D], BF16) for i in range(2)]
        scr1 = [ln_scr(sa1) for _ in range(2)]
        for g4 in range(4):
            b = g4 % 2
            P.dma("sp", lambda e, g4=g4, b=b: e.dma_start(out=xt[b], in_=x_d[g4 * 512:(g4 + 1) * 512, :].rearrange("(t p) d -> p t d", p=128)),
                  writes=[("xt", b)])
            for t in range(4):
                scr = scr1[t % 2]
                rr = ln_stats(xt[b][:, t, :], ("xt", b), scr)
                P.op("act", lambda e, b=b, t=t, scr=scr: e.activation(out=xn[b][:, t, :], in_=xt[b][:, t, :], func=AF.Identity,
                                                                      scale=scr[3], bias=scr[4]),
                     reads=[("xt", b)] + rr, writes=[("xn", b)])
            to_hT(xn[b], ("xn", b), g4, 1, 0, [4, 5, 6, 7])

        chk(1)
        P.scope("p2_inproj")
        dqT = view(Q_OFF, [128, 4, S], BF16)
        dkT = view(Q_OFF + 16 * KB, [128, 4, S], BF16)
        dv = view(Q_OFF + 32 * KB, [128, NT, 512], BF16)
        gqT = view(Q_OFF + 48 * KB, [128, 4, S], BF16)
        gkd = view(Q_OFF + 64 * KB, [128, 2, S], BF16)
        gv = view(Q_OFF + 72 * KB, [128, NT, 2, 128], BF16)
        P.barrier()
        sa2 = Alloc(S_OFF, ARENA_BYTES)
        wi = [sa2([128, 8, 512], BF16) for _ in range(4)] + [sa2([128, 8, 256], BF16)]
        t_sq = sa2([128, 8, 64], F32)
        t_xn = sa2([128, 8, 64], F32)
        t_t1 = sa2([128, 8, 64], F32)
        t_t2 = sa2([128, 8, 64], F32)
        t_ss = sa2([128, 8], F32)
        t_ln = sa2([128, 8], F32)
        t_rs = sa2([128, 8], F32)
        qrope = sa2([128, 4, 512], BF16)
        krope = sa2([128, 4, 256], BF16)

        def load_wi(ci, c0, ncol):
            b = ci
            P.dma("pool", lambda e: e.dma_start(out=wi[b][:, :, 0:ncol], in_=w_in_d[:, c0:c0 + ncol].rearrange("(c p) n -> p c n", p=128)),
                  writes=[("wi", b)])
            return b

        P.op("pool", lambda e: e.memset(gv[:, :, :, 64:128], 1.0), reads=[], writes=["gv_ones"])
        HT_ALL = [("hT", g) for g in range(4)]
        WB = {}
        WB[3] = load_wi(3, 1536, 512)
        WB[4] = load_wi(4, 2048, 256)
        WB[0] = load_wi(0, 0, 512)
        WB[1] = load_wi(1, 512, 512)
        WB[2] = load_wi(2, 1024, 512)
        heavy = []
        hv_rot = {"i": 0}

        def hv_bank():
            hv_rot["i"] += 1
            return 4 + hv_rot["i"] % 2

        for ci in range(2):
            b = WB[ci]
            dst = dqT if ci == 0 else dkT
            for blk in range(4):
                for tt in range(4):
                    def unit(ci=ci, b=b, blk=blk, tt=tt):
                        pb = hv_bank()
                        for c in range(8):
                            P.op("pe", lambda e, c=c, b=b, blk=blk, tt=tt, pb=pb: e.matmul(ps[pb], lhsT=wi[b][:, c, blk * 128:(blk + 1) * 128],
                                                                                         rhs=hT[:, c, tt * 512:(tt + 1) * 512],
                                                                                         start=(c == 0), stop=(c == 7)),
                                 reads=[("wi", b), ("hT", tt)], writes=[("ps", pb)], inc=(c == 7))
                        if ci == 0:
                            P.op("act", lambda e, blk=blk, tt=tt, pb=pb: e.activation(out=dqT[:, blk, tt * 512:(tt + 1) * 512], in_=ps[pb],
                                                                                     func=AF.Copy, scale=0.125),
                                 reads=[("ps", pb)], writes=[("dqT", blk)])
                        else:
                            P.op("act", lambda e, blk=blk, tt=tt, pb=pb: e.activation(out=dkT[:, blk, tt * 512:(tt + 1) * 512], in_=ps[pb],
                                                                                     func=AF.Copy),
                                 reads=[("ps", pb)], writes=[("dkT", blk)])
                    heavy.append(unit)
        b = WB[2]
        for t in range(NT):
            def unit(b=b, t=t):
                pb = hv_bank()
                for c in range(8):
                    P.op("pe", lambda e, c=c, b=b, t=t, pb=pb: e.matmul(ps[pb], lhsT=hT[:, c, t * 128:(t + 1) * 128], rhs=wi[b][:, c, :],
                                                                      start=(c == 0), stop=(c == 7)),
                         reads=[("wi", b), ("hT", t // 4)], writes=[("ps", pb)], inc=(c == 7))
                P.op("act", lambda e, t=t, pb=pb: e.activation(out=dv[:, t, :], in_=ps[pb], func=AF.Copy),
                     reads=[("ps", pb)], writes=["dv"])
            heavy.append(unit)

        def rms_rope(src, nh, t, gslice, out_writes):
            s3 = src.rearrange("p (h d) -> p h d", d=64)
            sq, xn_, t1, t2 = t_sq[:, 0:nh, :], t_xn[:, 0:nh, :], t_t1[:, 0:nh, :], t_t2[:, 0:nh, :]
            ss, ln_, rs = t_ss[:, 0:nh], t_ln[:, 0:nh], t_rs[:, 0:nh]
            srcres = out_writes[0][2]
            P.op("act", lambda e: e.activation(out=sq, in_=s3, func=AF.Square), reads=[srcres], writes=["t_sq"])
            P.op("dve", lambda e: e.tensor_reduce(out=ss, in_=sq, axis=AX.X, op=ALU.add), reads=["t_sq"], writes=["t_ss"])
            P.op("act", lambda e: e.activation(out=ln_, in_=ss, func=AF.Ln, bias=epsR, scale=1.0 / 64), reads=["t_ss", "eps"], writes=["t_ln"])
            P.op("act", lambda e: e.activation(out=rs, in_=ln_, func=AF.Exp, scale=-0.5), reads=["t_ln"], writes=["t_rs"])
            P.op("dve", lambda e: e.tensor_tensor(out=xn_, in0=s3, in1=rs.unsqueeze(2).to_broadcast([128, nh, 64]), op=ALU.mult),
                 reads=[srcres, "t_rs"], writes=["t_xn"])
            P.op("dve", lambda e: e.tensor_tensor(out=xn_, in0=xn_, in1=gslice, op=ALU.mult), reads=["t_xn", "gBq", "gBk"], writes=["t_xn"])
            cB = ropeC[:, t:t + 1, :].to_broadcast([128, nh, 64])
            P.op("dve", lambda e: e.tensor_tensor(out=t1, in0=xn_, in1=cB, op=ALU.mult), reads=["t_xn", "rope"], writes=["t_t1"])
            xn4 = xn_.rearrange("p h (i two) -> p h i two", two=2)
            t24 = t2.rearrange("p h (i two) -> p h i two", two=2)
            s4 = ropeS[:, t, :].rearrange("p (i two) -> p i two", two=2)
            P.op("dve", lambda e: e.tensor_tensor(out=t24[:, :, :, 0], in0=xn4[:, :, :, 1],
                                                  in1=s4[:, :, 0].unsqueeze(1).to_broadcast([128, nh, 32]), op=ALU.mult),
                 reads=["t_xn", "rope"], writes=["t_t2a"])
            P.op("dve", lambda e: e.tensor_tensor(out=t24[:, :, :, 1], in0=xn4[:, :, :, 0],
                                                  in1=s4[:, :, 1].unsqueeze(1).to_broadcast([128, nh, 32]), op=ALU.mult),
                 reads=["t_xn", "rope"], writes=["t_t2b"])
            for (oap, ores, _) in out_writes:
                P.op("dve", lambda e, oap=oap: e.tensor_tensor(out=oap, in0=t1, in1=t2, op=ALU.add),
                     reads=["t_t1", "t_t2a", "t_t2b"], writes=[ores])

        b3 = WB[3]
        b4 = WB[4]
        for g4 in range(4):
            for tl in range(4):
                t = g4 * 4 + tl
                pq = 0 + (t % 2)
                pk = 2 + (t % 2)
                for c in range(8):
                    P.op("pe", lambda e, c=c, t=t, pq=pq: e.matmul(ps[pq], lhsT=hT[:, c, t * 128:(t + 1) * 128], rhs=wi[b3][:, c, :],
                                                                  start=(c == 0), stop=(c == 7)),
                         reads=[("wi", b3), ("hT", g4)], writes=[("ps", pq)], inc=(c == 7))
                for c in range(8):
                    P.op("pe", lambda e, c=c, t=t, pk=pk: e.matmul(ps[pk][:, 0:256], lhsT=hT[:, c, t * 128:(t + 1) * 128], rhs=wi[b4][:, c, 0:256],
                                                                  start=(c == 0), stop=(c == 7)),
                         reads=[("wi", b4), ("hT", g4)], writes=[("ps", pk)], inc=(c == 7))
                P.op("act", lambda e, t=t, pk=pk: e.activation(out=gv[:, t, :, 0:64], in_=ps[pk][:, 128:256].rearrange("p (g d) -> p g d", d=64),
                                                              func=AF.Copy),
                     reads=[("ps", pk)], writes=["gv"])
                rms_rope(ps[pq], 8, t, gB[:, 0:8, :],
                         [(qrope[:, tl, :].rearrange("p (h d) -> p h d", d=64), ("qrope", tl), ("ps", pq))])
                k3 = krope[:, tl, :].rearrange("p (h d) -> p h d", d=64)
                rms_rope(ps[pk][:, 0:128], 2, t, gB[:, 8:10, :],
                         [(k3[:, 0:2, :], ("kropeA", tl), ("ps", pk))])
                P.op("dve", lambda e, k3=k3: e.tensor_copy(out=k3[:, 2, :], in_=k3[:, 1, :]), reads=[("kropeA", tl)],
                     writes=[("kropeB", tl)])
                P.op("dve", lambda e, k3=k3: e.tensor_copy(out=k3[:, 3, :], in_=k3[:, 0, :]), reads=[("kropeA", tl), ("kropeB", tl)],
                     writes=[("kropeB", tl)])
                for _ in range(3):
                    if heavy:
                        heavy.pop(0)()
            QR = [("qrope", tl) for tl in range(4)]
            KR = [("kropeA", tl) for tl in range(4)] + [("kropeB", tl) for tl in range(4)]
            for pair in range(4):
                pb = 6 + pair % 2
                for tl in range(4):
                    P.op("pe", lambda e, pair=pair, tl=tl, pb=pb: e.transpose(out=psbf[pb][:, tl * 128:(tl + 1) * 128],
                                                                             in_=qrope[:, tl, pair * 128:(pair + 1) * 128], identity=ident),
                         reads=QR + ["ident"], writes=[("ps", pb)], inc=(tl == 3))
                if pair % 2 == 0:
                    P.op("act", lambda e, pair=pair, pb=pb, g4=g4: e.activation(out=gqT[:, pair, g4 * 512:(g4 + 1) * 512], in_=psbf[pb][:, 0:512], func=AF.Copy),
                         reads=[("ps", pb)], writes=[("gqT", pair)])
                else:
                    P.op("dve", lambda e, pair=pair, pb=pb, g4=g4: e.tensor_copy(out=gqT[:, pair, g4 * 512:(g4 + 1) * 512], in_=psbf[pb][:, 0:512]),
                         reads=[("ps", pb)], writes=[("gqT", pair)])
            for ab in range(2):
                pb = 6 + ab
                for tl in range(4):
                    P.op("pe", lambda e, ab=ab, tl=tl, pb=pb: e.transpose(out=psbf[pb][:, tl * 128:(tl + 1) * 128],
                                                                         in_=krope[:, tl, ab * 128:(ab + 1) * 128], identity=ident),
                         reads=KR + ["ident"], writes=[("ps", pb)], inc=(tl == 3))
            sl = slice(g4 * 512, (g4 + 1) * 512)
            P.op("dve", lambda e, sl=sl: e.tensor_copy(out=gkd[0:64, 0, sl], in_=psbf[6][0:64, 0:512]), reads=[("ps", 6)], writes=[("gkd", 0)])
            P.op("dve", lambda e, sl=sl: e.tensor_copy(out=gkd[64:128, 1, sl], in_=psbf[6][64:128, 0:512]), reads=[("ps", 6)], writes=[("gkd", 1)])
            P.op("act", lambda e, sl=sl: e.activation(out=gkd[64:128, 0, sl], in_=psbf[7][64:128, 0:512], func=AF.Copy), reads=[("ps", 7)], writes=[("gkd", 0)])
            P.op("act", lambda e, sl=sl: e.activation(out=gkd[0:64, 1, sl], in_=psbf[7][0:64, 0:512], func=AF.Copy), reads=[("ps", 7)], writes=[("gkd", 1)])

        while heavy:
            heavy.pop(0)()
        chk(2)
        P.scope("p3_diff")
        P.barrier()
        sa3 = Alloc(S_OFF, ARENA_BYTES)
        NPT = 6
        pT = [sa3([128, 512], BF16) for _ in range(NPT)]
        btmp = [sa3([128, 384], F32) for _ in range(2)]
        e_r0 = sa3([128, 512], F32)
        e_r1 = sa3([128, 512], F32)
        e_a = sa3([128, 512], F32)
        e_b = sa3([128, 512], F32)
        e_sq = sa3([128, 512], BF16)
        e_ln = sa3([128, 512], F32)
        first_use = {"v": True}
        ztile = sa3([128, D], F32)
        P.op("pool", lambda e: e.memset(ztile, 0.0), writes=["ztile"])
        P.dma("sp", lambda e: e.dma_start(out=Xs_d.rearrange("(j p) d -> p j d", p=128), in_=ztile.unsqueeze(1).to_broadcast([128, NSL, D])),
              reads=["ztile"], writes=["zf_xs"])
        P.dma("sp", lambda e: e.dma_start(out=Hn_d.rearrange("(j p) d -> p j d", p=128),
                                          in_=ztile.bitcast(BF16)[:, 0:D].unsqueeze(1).to_broadcast([128, NSL, D])),
              reads=["ztile"], writes=["zf_hn"])
        zcw = sa3([128, 48], F32)
        P.op("pool", lambda e: e.memset(zcw[:, 0:32], 0.0), writes=["zcw"])
        P.op("pool", lambda e: e.memset(zcw[:, 32:48].bitcast(I32), 1 << 20), reads=["zcw"], writes=["zcw"])
        P.dma("sp", lambda e: e.dma_start(out=Cs_d.rearrange("(j p) d -> p j d", p=128), in_=zcw.unsqueeze(1).to_broadcast([128, NSL, 48])),
              reads=["zcw"], writes=["zf_cs"])

        def s3w(name):
            return [name]

        pt_rot = {"i": 0}
        bt_rot = {"i": 0}

        def exp_tile(h, c, kt, qt, sbank, diff):
            bi = pt_rot["i"] % NPT
            pt_rot["i"] += 1
            segs = []
            if diff and not DBG.get("nonear"):
                cur = None
                for i in range(4):
                    qs = 4 * qt + i
                    ty = "L" if qs < kt - 1 else ("R" if qs > kt + 1 else "N")
                    if DBG.get("nolr") and ty != "N":
                        ty = "Z"
                    if DBG.get("non") and ty == "N":
                        ty = "Z"
                    if cur is not None and cur[0] == ty:
                        cur[2] = i + 1
                    else:
                        cur = [ty, i, i + 1]
                        segs.append(cur)
            else:
                segs = [["Z", 0, 4]]
            for ty, a, b_ in segs:
                cs = slice(a * 128, b_ * 128)
                if ty == "N":
                    w = (b_ - a) * 128
                    off = (4 * qt + a - kt + 1) * 128
                    bt = bt_rot["i"] % 2
                    bt_rot["i"] += 1
                    P.op("dve", lambda e, cs=cs, w=w, off=off, bt=bt: e.tensor_tensor(out=btmp[bt][:, 0:w], in0=ps[sbank][:, cs],
                                                                                     in1=Tb[:, h, off:off + w], op=ALU.add),
                         reads=[("ps", sbank), "Tb"], writes=s3w(("btmp", bt)))
                    P.op("act", lambda e, cs=cs, w=w, bt=bt: e.activation(out=pT[bi][:, cs], in_=btmp[bt][:, 0:w], func=AF.Exp),
                         reads=[("btmp", bt)], writes=s3w(("pT", bi)))
                elif ty == "Z":
                    P.op("act", lambda e, cs=cs: e.activation(out=pT[bi][:, cs], in_=ps[sbank][:, cs], func=AF.Exp),
                         reads=[("ps", sbank)], writes=s3w(("pT", bi)))
                else:
                    col = 2 * h + (1 if ty == "L" else 0)
                    P.op("act", lambda e, cs=cs, col=col: e.activation(out=pT[bi][:, cs], in_=ps[sbank][:, cs], func=AF.Exp,
                                                                      bias=cbias[:, col:col + 1]),
                         reads=[("ps", sbank), "cbias"], writes=s3w(("pT", bi)))
            return bi

        def diff_qk(h, qt, kt, par):
            for c in range(2):
                sb_ = 2 * par + c
                P.op("pe", lambda e, c=c, sb_=sb_: e.matmul(ps[sb_], lhsT=dkT[c * 64:(c + 1) * 64, h, kt * 128:(kt + 1) * 128],
                                                            rhs=dqT[c * 64:(c + 1) * 64, h, qt * 512:(qt + 1) * 512], start=True, stop=True),
                     reads=[("dqT", h), ("dkT", h)], writes=[("ps", sb_)], inc=True)

        def diff_pv(h, qt, kt, bis):
            for c in range(2):
                P.op("pe", lambda e, c=c: e.matmul(ps[4 + c], lhsT=dv[:, kt, h * 128:(h + 1) * 128], rhs=pT[bis[c]],
                                                   start=(kt == 0), stop=(kt == NT - 1)),
                     reads=["dv", ("pT", bis[c])], writes=[("ps", 4 + c)], inc=(kt == NT - 1))
                P.op("pe", lambda e, c=c: e.matmul(ps[6 + c], lhsT=ones_bf, rhs=pT[bis[c]], start=(kt == 0), stop=(kt == NT - 1)),
                     reads=["ones", ("pT", bis[c])], writes=[("ps", 6 + c)], inc=True)

        def diff_epi_head(h, qt):
            P.op("act", lambda e: e.activation(out=e_r0, in_=ps[6], func=AF.Ln), reads=[("ps", 6)], writes=s3w("e_r0"))
            P.op("act", lambda e: e.activation(out=e_r0, in_=e_r0, func=AF.Exp, scale=-1.0), reads=["e_r0"], writes=["e_r0"])
            P.op("dve", lambda e: e.tensor_tensor(out=e_a, in0=ps[4], in1=e_r0, op=ALU.mult), reads=[("ps", 4), "e_r0"], writes=s3w("e_a"))
            P.op("act", lambda e: e.activation(out=e_r1, in_=ps[7], func=AF.Ln), reads=[("ps", 7)], writes=s3w("e_r1"))
            P.op("act", lambda e: e.activation(out=e_r1, in_=e_r1, func=AF.Exp, scale=-1.0), reads=["e_r1"], writes=["e_r1"])
            P.op("dve", lambda e: e.tensor_tensor(out=e_b, in0=ps[5], in1=e_r1, op=ALU.mult), reads=[("ps", 5), "e_r1"], writes=s3w("e_b"))
            P.op("dve", lambda e: e.scalar_tensor_tensor(out=e_a, in0=e_b, scalar=nlam, in1=e_a, op0=ALU.mult, op1=ALU.add),
                 reads=["e_a", "e_b", "nlam"], writes=["e_a"])

        def diff_epi_tail(h, qt, bank):
            qs = slice(qt * 512, (qt + 1) * 512)
            P.op("act", lambda e: e.activation(out=e_sq, in_=e_a, func=AF.Square), reads=["e_a"], writes=s3w("e_sq"))
            P.op("pe", lambda e: e.matmul(ps[bank], lhsT=ones_bf, rhs=e_sq, start=True, stop=True), reads=["ones", "e_sq"], writes=[("ps", bank)])
            P.op("act", lambda e: e.activation(out=e_ln, in_=ps[bank], func=AF.Ln, bias=epsR, scale=1.0 / 128), reads=[("ps", bank), "eps"],
                 writes=s3w("e_ln"))
            P.op("act", lambda e: e.activation(out=e_ln, in_=e_ln, func=AF.Exp, scale=-0.5), reads=["e_ln"], writes=["e_ln"])
            P.op("dve", lambda e: e.scalar_tensor_tensor(out=mixT[:, h, qs], in0=e_a, scalar=gsub, in1=e_ln, op0=ALU.mult, op1=ALU.mult),
                 reads=["e_a", "e_ln", "gsub"], writes=[("hT", qt)])

        items = [(h, qt, kt) for h in range(4) for qt in range(4) for kt in range(NT)]
        items = items[:DBG.get("diff_n", len(items))]
        pend_tail = None
        for idx, (h, qt, kt) in enumerate(items):
            if idx == 0:
                diff_qk(h, qt, kt, 0)
            if idx + 1 < len(items):
                h2, qt2, kt2 = items[idx + 1]
                diff_qk(h2, qt2, kt2, (idx + 1) % 2)
            par = idx % 2
            bis = [exp_tile(h, c, kt, qt, 2 * par + c, True) for c in range(2)]
            if not DBG.get("nopv"):
                diff_pv(h, qt, kt, bis)
            if pend_tail is not None and idx >= pend_tail[0]:
                diff_epi_tail(pend_tail[1], pend_tail[2], 2 * par)
                pend_tail = None
            if kt == NT - 1 and not DBG.get("noepi"):
                diff_epi_head(h, qt)
                pend_tail = (idx + 2, h, qt)
                first_use["v"] = False
        if pend_tail is not None:
            diff_epi_tail(pend_tail[1], pend_tail[2], 0)
            pend_tail = None

        chk(2.5)
        P.scope("p3_gqa")
        g_rt = e_r0
        g_ot = e_a

        def gqa_qk(pair, qt, kt, par):
            g = pair // 2
            for c in range(2):
                sb_ = 2 * par + c
                P.op("pe", lambda e, c=c, sb_=sb_: e.matmul(ps[sb_], lhsT=gkd[c * 64:(c + 1) * 64, g, kt * 128:(kt + 1) * 128],
                                                            rhs=gqT[c * 64:(c + 1) * 64, pair, qt * 512:(qt + 1) * 512], start=True, stop=True),
                     reads=[("gqT", pair), ("gkd", g)], writes=[("ps", sb_)], inc=True)

        def gqa_pv(pair, qt, kt, bis):
            g = pair // 2
            for c in range(2):
                P.op("pe", lambda e, c=c: e.matmul(ps[4 + c], lhsT=gvl[c][:, kt, g, :], rhs=pT[bis[c]], start=(kt == 0), stop=(kt == NT - 1)),
                     reads=["gv", "gv_ones", "gvB", ("pT", bis[c])], writes=[("ps", 4 + c)], inc=(c == 1 or kt == NT - 1))

        gvB = view(Q_OFF, [128, NT, 2, 128], BF16)
        P.alias("GVB", [("dqT", i) for i in range(4)])
        P.op("dve", lambda e: e.tensor_copy(out=gvB[:, :, :, 64:128], in_=gv[:, :, :, 0:64]), reads=["gv"], writes=["GVB", "gvB"])
        P.op("dve", lambda e: e.tensor_copy(out=gvB[:, :, :, 0:64], in_=gv[:, :, :, 64:128]), reads=["gv_ones", "gvB"], writes=["gvB"])
        gvl = [gv, gvB]

        def gqa_epi_head(pair, qt):
            P.op("act", lambda e: e.activation(out=g_rt[64:128, :], in_=ps[4][64:128, :], func=AF.Ln), reads=[("ps", 4)], writes=["e_r0"])
            P.op("dve", lambda e: e.tensor_copy(out=g_ot[0:64, :], in_=ps[4][0:64, :]), reads=[("ps", 4)], writes=["e_a"])
            P.op("act", lambda e: e.activation(out=g_rt[0:64, :], in_=ps[5][0:64, :], func=AF.Ln), reads=[("ps", 5)], writes=["e_r0"])
            P.op("dve", lambda e: e.tensor_copy(out=g_ot[64:128, :], in_=ps[5][64:128, :]), reads=[("ps", 5)], writes=["e_a"])
            P.op("act", lambda e: e.activation(out=g_rt, in_=g_rt, func=AF.Exp, scale=-1.0), reads=["e_r0"], writes=["e_r0"])

        def gqa_epi_tail(pair, qt, bank):
            qs = slice(qt * 512, (qt + 1) * 512)
            P.op("pe", lambda e: e.matmul(ps[bank], lhsT=swapM, rhs=g_rt, start=True, stop=True), reads=["swapM", "e_r0"], writes=[("ps", bank)])
            P.op("dve", lambda e: e.tensor_tensor(out=mixT[:, 4 + pair, qs], in0=g_ot, in1=ps[bank], op=ALU.mult),
                 reads=["e_a", ("ps", bank)], writes=[("hT", qt)])

        items = [(pair, qt, kt) for pair in range(4) for qt in range(4) for kt in range(NT)]
        pend_tail = None
        n_epi = 0
        for idx, (pair, qt, kt) in enumerate(items):
            if idx == 0:
                gqa_qk(pair, qt, kt, 0)
            if idx + 1 < len(items):
                p2, qt2, kt2 = items[idx + 1]
                gqa_qk(p2, qt2, kt2, (idx + 1) % 2)
            par = idx % 2
            bis = [exp_tile(0, c, kt, qt, 2 * par + c, False) for c in range(2)]
            gqa_pv(pair, qt, kt, bis)
            if pend_tail is not None and idx >= pend_tail[0]:
                gqa_epi_tail(pend_tail[1], pend_tail[2], 6 + (pend_tail[3] % 2))
                pend_tail = None
            if kt == NT - 1:
                gqa_epi_head(pair, qt)
                pend_tail = (idx + 2, pair, qt, n_epi)
                n_epi += 1
        if pend_tail is not None:
            gqa_epi_tail(pend_tail[1], pend_tail[2], 6)
            pend_tail = None

        chk(3)
        P.scope("p4_outproj_ln1")
        P.barrier()
        sa4 = Alloc(S_OFF, ARENA_BYTES)
        wo = [sa4([128, 8, 512], BF16) for _ in range(2)]
        xre = [sa4([128, D], F32) for _ in range(4)]
        lnB = sa4([128, 2, D], F32)
        xn2 = sa4([128, 4, D], BF16)
        scr4 = [ln_scr(sa4) for _ in range(4)]
        scr4b = [ln_scr(sa4) for _ in range(4)]
        first4 = {"v": True}

        def s4w(names):
            return list(names) + (["S4"])

        for hf in range(2):
            P.dma("pool", lambda e, hf=hf: e.dma_start(out=wo[hf], in_=w_out_d[:, hf * 512:(hf + 1) * 512].rearrange("(c p) n -> p c n", p=128)),
                  writes=[("wo", hf)])
        P.dma("sp", lambda e: e.dma_start(out=lnB[:, 0, :], in_=ln1g_d.partition_broadcast(128)), writes=["lnB0"])
        P.dma("sp", lambda e: e.dma_start(out=lnB[:, 1, :], in_=ln1b_d.partition_broadcast(128)), writes=["lnB1"])

        keep = []
        STAT2 = {}
        for g4 in range(4):
            tiles = [g4 * 4 + tl for tl in range(4)]
            for tl, t in enumerate(tiles):
                xb = tl
                P.dma("sp", lambda e, t=t, xb=xb: e.dma_start(out=xre[xb], in_=x_d[t * 128:(t + 1) * 128, :]), writes=[("xre", xb)])
            for tl, t in enumerate(tiles):
                xb = tl
                for hf in range(2):
                    pb = 2 * (t % 2) + hf
                    for c in range(8):
                        P.op("pe", lambda e, c=c, t=t, hf=hf, pb=pb: e.matmul(ps[pb], lhsT=mixT[:, c, t * 128:(t + 1) * 128], rhs=wo[hf][:, c, :],
                                                                            start=(c == 0), stop=(c == 7)),
                             reads=[("hT", g4), ("wo", hf)], writes=[("ps", pb)], inc=(c == 7))
                    P.op("dve", lambda e, hf=hf, pb=pb, t=t: e.tensor_tensor(out=Xr[:, t, hf * 512:(hf + 1) * 512], in0=ps[pb],
                                                                            in1=g1B[:, hf * 512:(hf + 1) * 512], op=ALU.mult),
                         reads=[("ps", pb)] + G1B, writes=[("X", t)])
                xrow = Xr[:, t, :]
                P.op("dve", lambda e, xb=xb, xrow=xrow: e.scalar_tensor_tensor(out=xrow, in0=xre[xb], scalar=ALPHA, in1=xrow,
                                                                              op0=ALU.mult, op1=ALU.add),
                     reads=[("xre", xb), ("X", t)], writes=[("X", t)])
            rr1 = {}
            for tl, t in enumerate(tiles):
                rr1[t] = ln_stats(Xr[:, t, :], ("X", t), scr4[tl])
            for tl, t in enumerate(tiles):
                xrow = Xr[:, t, :]
                scr = scr4[tl]
                P.op("act", lambda e, xrow=xrow, scr=scr: e.activation(out=xrow, in_=xrow, func=AF.Identity, scale=scr[3], bias=scr[4]),
                     reads=[("X", t)] + rr1[t], writes=[("X", t)])
            for tl, t in enumerate(tiles):
                xrow = Xr[:, t, :]
                P.op("dve", lambda e, xrow=xrow: e.tensor_tensor(out=xrow, in0=xrow, in1=lnB[:, 0, :], op=ALU.mult),
                     reads=[("X", t), "lnB0"], writes=[("X", t)])
            for tl, t in enumerate(tiles):
                xrow = Xr[:, t, :]
                P.op("dve", lambda e, xrow=xrow: e.tensor_tensor(out=xrow, in0=xrow, in1=lnB[:, 1, :], op=ALU.add),
                     reads=[("X", t), "lnB1"], writes=[("X", t)])
            for tl, t in enumerate(tiles):
                sb_ = scr4b[tl]
                scr = (sb_[0], sb_[1], sb_[2], rs2[:, t:t + 1], nm2[:, t:t + 1])
                keep.append(scr)
                STAT2[t] = ln_stats(Xr[:, t, :], ("X", t), scr)
            for tl, t in enumerate(tiles):
                xrow = Xr[:, t, :]
                P.op("act", lambda e, xrow=xrow, t=t, tl=tl: e.activation(out=xn2[:, tl, :], in_=xrow, func=AF.Identity, scale=rs2[:, t:t + 1],
                                                                         bias=nm2[:, t:t + 1]),
                     reads=[("X", t)] + STAT2[t], writes=["xn2"])
            to_hT(xn2, "xn2", g4, 3, 2, [4, 5, 6, 7])

        chk(3.5)
        P.scope("p4b_router_sort")
        P.barrier()
        sa4 = Alloc(S_OFF, ARENA_BYTES)
        wr = sa4([128, 8, 36], BF16)
        brB = sa4([128, 36], F32)
        P.dma("pool", lambda e: e.dma_start(out=wr, in_=wr_d.rearrange("(c p) n -> p c n", p=128)), writes=["wr"])
        P.dma("sp", lambda e: e.dma_start(out=brB, in_=br_d.partition_broadcast(128)), writes=["brB"])
        rl = sa4([128, NT, 36], F32)
        for t in range(NT):
            pb = t // 8
            o = (t % 8) * 36
            for c in range(8):
                P.op("pe", lambda e, c=c, t=t, pb=pb, o=o: e.matmul(ps[pb][:, o:o + 36], lhsT=hT[:, c, t * 128:(t + 1) * 128], rhs=wr[:, c, :],
                                                                  start=(c == 0), stop=(c == 7)),
                     reads=[("hT", t // 4), "wr"], writes=[("ps", pb)], inc=(c == 7))
        for pb in range(2):
            P.op("dve", lambda e, pb=pb: e.tensor_tensor(out=rl[:, pb * 8:(pb + 1) * 8, :], in0=ps[pb][:, 0:288].rearrange("p (t n) -> p t n", n=36),
                                                         in1=brB.unsqueeze(1).to_broadcast([128, 8, 36]), op=ALU.add),
                 reads=[("ps", pb), "brB"], writes=["rl" + str(pb)])
        RL = ["rl0", "rl1"]
        gl = rl[:, :, 0:4]
        el = rl[:, :, 4:36].rearrange("p t (g e) -> p t g e", e=8)
        r_gmax = sa4([128, NT], F32)
        r_oh = sa4([128, NT, 4], F32)
        r_ex = sa4([128, NT, 4], F32)
        r_sum = sa4([128, NT], F32)
        r_pg = sa4([128, NT], F32)
        r_em = sa4([128, NT, 4, 8], F32)
        r_es = sa4([128, NT, 8], F32)
        r_m1 = sa4([128, NT], F32)
        r_k1 = sa4([128, NT, 8], F32)
        r_e2 = sa4([128, NT, 8], F32)
        r_m2 = sa4([128, NT], F32)
        r_k2 = sa4([128, NT, 8], F32)
        r_d = sa4([128, NT], F32)
        r_w1 = sa4([128, NT], F32)
        r_w2 = sa4([128, NT], F32)
        r_cs = sa4([128, NT, 8], F32)

        def dv_(fn, reads, writes):
            P.op("dve", fn, reads=reads, writes=writes)

        dv_(lambda e: e.tensor_reduce(out=r_gmax, in_=gl, axis=AX.X, op=ALU.max), RL, ["r_gmax"])
        dv_(lambda e: e.tensor_tensor(out=r_oh, in0=gl, in1=r_gmax.unsqueeze(2).to_broadcast([128, NT, 4]), op=ALU.is_equal),
            RL + ["r_gmax"], ["r_oh"])
        dv_(lambda e: e.tensor_tensor(out=r_ex, in0=gl, in1=r_gmax.unsqueeze(2).to_broadcast([128, NT, 4]), op=ALU.subtract),
            RL + ["r_gmax"], ["r_ex"])
        P.op("act", lambda e: e.activation(out=r_ex, in_=r_ex, func=AF.Exp), reads=["r_ex"], writes=["r_ex"])
        dv_(lambda e: e.tensor_reduce(out=r_sum, in_=r_ex, axis=AX.X, op=ALU.add), ["r_ex"], ["r_sum"])
        dv_(lambda e: e.reciprocal(out=r_pg, in_=r_sum), ["r_sum"], ["r_pg"])
        dv_(lambda e: e.tensor_tensor(out=r_em, in0=el, in1=r_oh.unsqueeze(3).to_broadcast([128, NT, 4, 8]), op=ALU.mult),
            RL + ["r_oh"], ["r_em"])
        dv_(lambda e: e.tensor_reduce(out=r_es, in_=r_em.rearrange("p t g e -> p t e g"), axis=AX.X, op=ALU.add), ["r_em"], ["r_es"])
        dv_(lambda e: e.tensor_reduce(out=r_m1, in_=r_es, axis=AX.X, op=ALU.max), ["r_es"], ["r_m1"])
        dv_(lambda e: e.tensor_tensor(out=r_k1, in0=r_es, in1=r_m1.unsqueeze(2).to_broadcast([128, NT, 8]), op=ALU.is_equal),
            ["r_es", "r_m1"], ["r_k1"])
        dv_(lambda e: e.scalar_tensor_tensor(out=r_e2, in0=r_k1, scalar=-1e30, in1=r_es, op0=ALU.mult, op1=ALU.add),
            ["r_k1", "r_es"], ["r_e2"])
        dv_(lambda e: e.tensor_reduce(out=r_m2, in_=r_e2, axis=AX.X, op=ALU.max), ["r_e2"], ["r_m2"])
        dv_(lambda e: e.tensor_tensor(out=r_k2, in0=r_e2, in1=r_m2.unsqueeze(2).to_broadcast([128, NT, 8]), op=ALU.is_equal),
            ["r_e2", "r_m2"], ["r_k2"])
        dv_(lambda e: e.tensor_tensor(out=r_d, in0=r_m2, in1=r_m1, op=ALU.subtract), ["r_m1", "r_m2"], ["r_d"])
        P.op("act", lambda e: e.activation(out=r_d, in_=r_d, func=AF.Exp), reads=["r_d"], writes=["r_d"])
        dv_(lambda e: e.tensor_scalar(out=r_w1, in0=r_d, scalar1=1.0, scalar2=None, op0=ALU.add), ["r_d"], ["r_w1"])
        dv_(lambda e: e.reciprocal(out=r_w1, in_=r_w1), ["r_w1"], ["r_w1"])
        dv_(lambda e: e.tensor_tensor(out=r_w1, in0=r_w1, in1=r_pg, op=ALU.mult), ["r_w1", "r_pg"], ["r_w1"])
        dv_(lambda e: e.tensor_tensor(out=r_w2, in0=r_w1, in1=r_d, op=ALU.mult), ["r_w1", "r_d"], ["r_w2"])
        dv_(lambda e: e.tensor_tensor(out=r_k1, in0=r_k1, in1=r_w1.unsqueeze(2).to_broadcast([128, NT, 8]), op=ALU.mult),
            ["r_k1", "r_w1"], ["r_k1"])
        dv_(lambda e: e.tensor_tensor(out=r_k2, in0=r_k2, in1=r_w2.unsqueeze(2).to_broadcast([128, NT, 8]), op=ALU.mult),
            ["r_k2", "r_w2"], ["r_k2"])
        dv_(lambda e: e.tensor_tensor(out=r_cs, in0=r_k1, in1=r_k2, op=ALU.add), ["r_k1", "r_k2"], ["r_cs"])
        cw48 = sa4([128, NT, 48], F32)
        P.op("pool", lambda e: e.iota(cw48[:, :, 32:48].bitcast(I32), pattern=[[128, NT], [0, 16]], base=0, channel_multiplier=1),
             writes=["cw_tok"])
        cw4 = cw48[:, :, 0:32].rearrange("p t (g e) -> p t g e", e=8)
        dv_(lambda e: e.tensor_tensor(out=cw4, in0=r_oh.unsqueeze(3).to_broadcast([128, NT, 4, 8]),
                                      in1=r_cs.unsqueeze(2).to_broadcast([128, NT, 4, 8]), op=ALU.mult),
            ["r_oh", "r_cs"], ["cw"])

        chk(4)
        P.scope("p5_moe")
        ohb = sa4([128, NT, 4], BF16)
        ltri = sa4([128, 128], BF16)
        wth = sa4([128, NT, 4], F32)
        csum = sa4([128, NT, 4], F32)
        cum = sa4([128, NT, 4], F32)
        ntot = sa4([128, 4], F32)
        nti = sa4([128, 4], F32).bitcast(I32)
        ntf = sa4([128, 4], F32)
        tsf = sa4([128, 8], F32)
        segs = sa4([128, 4], F32)
        ptmp = sa4([128, NT, 4], F32)
        posf = sa4([128, NT], F32)
        xsb = [sa4([128, D], BF16) for _ in range(2)]
        P.op("pool", lambda e: e.memset(ltri, 1.0), writes=["ltri"])
        P.op("pool", lambda e: e.affine_select(out=ltri, in_=ltri, pattern=[[1, 128]], compare_op=ALU.is_gt, fill=0.0, base=0,
                                               channel_multiplier=-1), reads=["ltri"], writes=["ltri"])
        dv_(lambda e: e.tensor_copy(out=ohb, in_=r_oh), ["r_oh"], ["ohb"])
        oh2 = ohb.rearrange("p t g -> p (t g)")
        P.op("pe", lambda e: e.matmul(ps[2][:, 0:64], lhsT=ltri, rhs=oh2, start=True, stop=True), reads=["ltri", "ohb"], writes=[("ps", 2)])
        P.op("pe", lambda e: e.matmul(ps[3][:, 0:64], lhsT=ones_bf, rhs=oh2, start=True, stop=True), reads=["ones", "ohb"], writes=[("ps", 3)])
        dv_(lambda e: e.tensor_copy(out=wth.rearrange("p t g -> p (t g)"), in_=ps[2][:, 0:64]), [("ps", 2)], ["wth"])
        dv_(lambda e: e.tensor_copy(out=csum.rearrange("p t g -> p (t g)"), in_=ps[3][:, 0:64]), [("ps", 3)], ["csum"])
        dv_(lambda e: e.memset(cum[:, 0, :], 0.0), [], ["cum"])
        for tt in range(1, NT):
            dv_(lambda e, tt=tt: e.tensor_tensor(out=cum[:, tt, :], in0=cum[:, tt - 1, :], in1=csum[:, tt - 1, :], op=ALU.add),
                ["cum", "csum"], ["cum"])
        dv_(lambda e: e.tensor_tensor(out=ntot, in0=cum[:, NT - 1, :], in1=csum[:, NT - 1, :], op=ALU.add), ["cum", "csum"], ["ntot"])
        dv_(lambda e: e.tensor_scalar(out=ntf, in0=ntot, scalar1=127.0, scalar2=None, op0=ALU.add), ["ntot"], ["ntf"])
        dv_(lambda e: e.tensor_copy(out=nti, in_=ntf), ["ntf"], ["nti"])
        dv_(lambda e: e.tensor_single_scalar(out=nti, in_=nti, scalar=7, op=ALU.arith_shift_right), ["nti"], ["nti"])
        dv_(lambda e: e.tensor_copy(out=ntf, in_=nti), ["nti"], ["ntf"])
        dv_(lambda e: e.memset(tsf[:, 0:1], 0.0), [], ["tsf"])
        for g in range(1, 4):
            dv_(lambda e, g=g: e.tensor_tensor(out=tsf[:, g:g + 1], in0=tsf[:, g - 1:g], in1=ntf[:, g - 1:g], op=ALU.add), ["tsf", "ntf"], ["tsf"])
        dv_(lambda e: e.tensor_tensor(out=tsf[:, 4:8], in0=tsf[:, 0:4], in1=ntf, op=ALU.add), ["tsf", "ntf"], ["tsf"])
        dv_(lambda e: e.tensor_copy(out=tsi, in_=tsf), ["tsf"], ["tsi"])
        dv_(lambda e: e.tensor_scalar(out=segs, in0=tsf[:, 0:4], scalar1=128.0, scalar2=None, op0=ALU.mult), ["tsf"], ["segs"])
        dv_(lambda e: e.tensor_tensor(out=ptmp, in0=cum, in1=wth, op=ALU.add), ["cum", "wth"], ["ptmp"])
        dv_(lambda e: e.tensor_tensor(out=ptmp, in0=ptmp, in1=segs.unsqueeze(1).to_broadcast([128, NT, 4]), op=ALU.add), ["ptmp", "segs"], ["ptmp"])
        dv_(lambda e: e.tensor_tensor(out=ptmp, in0=ptmp, in1=r_oh, op=ALU.mult), ["ptmp", "r_oh"], ["ptmp"])
        dv_(lambda e: e.tensor_reduce(out=posf, in_=ptmp, axis=AX.X, op=ALU.add), ["ptmp"], ["posf"])
        dv_(lambda e: e.tensor_copy(out=posi, in_=posf), ["posf"], ["posi"])

        def scat(dst, src, t):
            def f(e):
                try:
                    return e.indirect_dma_start(out=dst, out_offset=bass.IndirectOffsetOnAxis(ap=posi[:, t:t + 1], axis=0),
                                                in_=src, in_offset=None)
                except Exception:
                    print("SCAT FAIL", dst, src, posi[:, t:t + 1], t)
                    raise
            return f

        sc_toks = []
        for t in range(NT):
            xrow = Xr[:, t, :]
            xb = t % 2
            P.op("act", lambda e, xrow=xrow, xb=xb, t=t: e.activation(out=xsb[xb], in_=xrow, func=AF.Identity, scale=rs2[:, t:t + 1],
                                                                     bias=nm2[:, t:t + 1]),
                 reads=[("X", t)] + STAT2[t], writes=[("xsb", xb)])
            sc_toks.append(P.dma("pool", scat(Hn_d[:, :], xsb[xb], t), reads=[("xsb", xb), "posi", "zf_hn"], writes=[("hn_d", t)]))
            P.op("dve", lambda e, xrow=xrow: e.tensor_scalar(out=xrow, in0=xrow, scalar1=ALPHA, scalar2=None, op0=ALU.mult),
                 reads=[("X", t)], writes=[("X", t)])
            sc_toks.append(P.dma("pool", scat(Xs_d[:, :], xrow, t), reads=[("X", t), "posi", "zf_xs"], writes=[("xs_d", t)]))
            sc_toks.append(P.dma("pool", scat(Cs_d[:, :], cw48[:, t, :], t), reads=["cw", "cw_tok", "posi", "zf_cs"], writes=[("cs_d", t)]))

        chk(4)
        P.barrier()
        TB_OFF = 512 + 256 + 256 + 512
        ov = Alloc(TB_OFF, TB_OFF + 6144)
        hidb = ov([128, 4, 128], BF16)
        hidt = [ov([128, 512], BF16) for _ in range(2)]
        sgt5 = ov([128, 512], BF16)
        cws = view(TB_OFF + 6144 + 8192, [128, NSL, 48], F32)
        lnB2 = view(TB_OFF + 6144, [128, 2, D], F32)
        sa5 = Alloc(S_OFF, ARENA_BYTES)
        Wg = [sa5([128, 8, 512], BF16) for _ in range(2)]
        Wu = [sa5([128, 8, 512], BF16) for _ in range(2)]
        Wd = [sa5([128, 4, D], BF16) for _ in range(2)]
        hn4 = Wd[1].rearrange("p a b -> p (a b)")[:, 0:4 * D].rearrange("p (a b) -> p a b", b=D)
        scr6 = [ln_scr(sa5) for _ in range(2)]
        P.dma("sp", lambda e: e.dma_start(out=lnB2[:, 0, :], in_=ln2g_d.partition_broadcast(128)), writes=["ln2B0"])
        P.dma("sp", lambda e: e.dma_start(out=lnB2[:, 1, :], in_=ln2b_d.partition_broadcast(128)), writes=["ln2B1"])
        pre_toks = [P.dma("sp", lambda e: e.dma_start(out=cws, in_=Cs_d.rearrange("(j p) n -> p j n", p=128)), writes=["cws"])]
        for j4 in range(NSL // 4):
            pre_toks.append(P.dma("sp", lambda e, j4=j4: e.dma_start(out=Xr[:, j4 * 4:(j4 + 1) * 4, :],
                                                                     in_=Xs_d[j4 * 512:(j4 + 1) * 512, :].rearrange("(j p) d -> p j d", p=128)),
                                  writes=[("Xs", j4 * 4 + i) for i in range(4)]))
        for j4 in range(NSL // 4):
            P.dma("sp", lambda e, j4=j4: e.dma_start(out=hn4, in_=Hn_d[j4 * 512:(j4 + 1) * 512, :].rearrange("(j p) d -> p j d", p=128)),
                  writes=[("Wd", 1)])
            to_hT(hn4, ("Wd", 1), j4, 3, 2, [4, 5, 6, 7], hT=hTs, hres="hTs")

        def load_expert(ex):
            b = ex % 2
            P.dma("pool", lambda e: e.dma_start(out=Wg[b], in_=wg_d[ex].rearrange("(c p) n -> p c n", p=128)), writes=[("Wg", b)])
            P.dma("pool", lambda e: e.dma_start(out=Wu[b], in_=wu_d[ex].rearrange("(c p) n -> p c n", p=128)), writes=[("Wu", b)])
            P.dma("pool", lambda e: e.dma_start(out=Wd[b], in_=wd_d[ex].rearrange("(c p) n -> p c n", p=128)), writes=[("Wd", b)])
            P.op("pool", lambda e: e.tensor_tensor(out=Wd[b], in0=Wd[b], in1=g2B.unsqueeze(1).to_broadcast([128, 4, D]), op=ALU.mult),
                 reads=[("Wd", b)] + G2B, writes=[("Wd", b)])

        def moe_gu(ex, j):
            b = ex % 2
            par = j % 2
            pg, pu = par, 2 + par
            js = slice(j * 128, (j + 1) * 128)
            hres = ("hTs", j // 4)
            for c in range(8):
                P.op("pe", lambda e, c=c: e.matmul(ps[pg], lhsT=hTs[:, c, js], rhs=Wg[b][:, c, :], start=(c == 0), stop=(c == 7)),
                     reads=[("Wg", b), hres], writes=[("ps", pg)], inc=(c == 7))
            for c in range(8):
                P.op("pe", lambda e, c=c: e.matmul(ps[pu], lhsT=hTs[:, c, js], rhs=Wu[b][:, c, :], start=(c == 0), stop=(c == 7)),
                     reads=[("Wu", b), hres], writes=[("ps", pu)], inc=(c == 7))
            P.op("act", lambda e: e.activation(out=sgt5, in_=ps[pg], func=AF.Silu), reads=[("ps", pg)], writes=["sgt5"])
            P.op("dve", lambda e: e.tensor_tensor(out=hidt[par], in0=ps[pu], in1=sgt5, op=ALU.mult),
                 reads=[("ps", pu), "sgt5"], writes=[("hidt", par)])

        def moe_t(ex, j):
            par = j % 2
            for fc in range(4):
                P.op("pe", lambda e, fc=fc: e.transpose(out=psbf[6][:, fc * 128:(fc + 1) * 128], in_=hidt[par][:, fc * 128:(fc + 1) * 128],
                                                        identity=ident),
                     reads=[("hidt", par), "ident"], writes=[("ps", 6)], inc=(fc == 3))
            P.op("dve", lambda e: e.tensor_copy(out=hidb.rearrange("p a b -> p (a b)"), in_=psbf[6][:, 0:512]),
                 reads=[("ps", 6)], writes=["hidb"])

        def moe_y(ex, j):
            b = ex % 2
            for fc in range(4):
                for hf in range(2):
                    pb = 4 + hf
                    P.op("pe", lambda e, fc=fc, hf=hf, pb=pb: e.matmul(ps[pb], lhsT=hidb[:, fc, :], rhs=Wd[b][:, fc, hf * 512:(hf + 1) * 512],
                                                                      start=(fc == 0), stop=(fc == 3)),
                         reads=["hidb", ("Wd", b)], writes=[("ps", pb)], inc=(fc == 3))
            for hf in range(2):
                pb = 4 + hf
                xs_ = Xr[:, j, hf * 512:(hf + 1) * 512]
                P.op("dve", lambda e, xs_=xs_, pb=pb: e.scalar_tensor_tensor(out=xs_, in0=ps[pb], scalar=cws[:, j, ex:ex + 1], in1=xs_,
                                                                           op0=ALU.mult, op1=ALU.add),
                     reads=[("ps", pb), "cws", ("Xs", j)], writes=[("Xs", j)])

        for eng_ in ("pe", "act", "dve"):
            P.wait_all(eng_, pre_toks)
        P.op("pe", lambda e: e.matmul(ps[7][0:1, 0:2], lhsT=ident[0:1, 0:1], rhs=ident[0:1, 0:2], start=True, stop=True),
             reads=["ident"], writes=[("ps", 7)])
        for g in range(4):
            P.vload("ts%d" % g, tsi[0:1, g:g + 1], ["tsi"])
            P.vload("te%d" % g, tsi[0:1, 4 + g:5 + g], ["tsi"])
        for ex in range(N_MOE_EXPERTS):
            g = ex // 8
            load_expert(ex)
            asc = g < 2
            TS, TE = "ts%d" % g, "te%d" % g
            NCH = 19
            def c_lo_le(k, asc=asc, TS=TS, TE=TE):
                if asc:
                    return lambda v, k=k: v[TS] <= k
                return lambda v, k=k: v[TE] >= NCH - k
            def c_hi_gt(k, asc=asc, TS=TS, TE=TE):
                if asc:
                    return lambda v, k=k: v[TE] > k
                return lambda v, k=k: v[TS] < NCH - k
            def tile(k, asc=asc):
                return k if asc else NCH - 1 - k
            depth = 0
            for k in range(NCH + 1):
                if k >= 1:
                    P.begin_if(c_hi_gt(k - 1))
                    depth += 1
                P.begin_if(c_lo_le(k) if (asc and g > 0) or not asc else (lambda v, TE=TE: v[TE] >= 0))
                if k >= 1:
                    P.begin_if(c_lo_le(k - 1))
                    moe_t(ex, tile(k - 1))
                    P.end_if()
                if k < NCH:
                    P.begin_if(c_hi_gt(k))
                    moe_gu(ex, tile(k))
                    P.end_if()
                if k >= 1:
                    P.begin_if(c_lo_le(k - 1))
                    moe_y(ex, tile(k - 1))
                    P.end_if()
                P.end_if()
            for _ in range(depth):
                P.end_if()
        chk(5)
        stopped = False
    except _Stop:
        P.barrier()
        stopped = True

    P.scope("p6_ln2_out")
    if stopped:
        otoks = []
        for t in range(NT):
            otoks.append(P.dma("sp", lambda e, t=t: e.dma_start(out=y_d[t * 128:(t + 1) * 128, :], in_=Xr[:, t, :]), reads=[("X", t)]))
    else:
        stoks = []
        otoks = []
        scr6l = [ln_scr(sa5) for _ in range(4)]
        for j4 in range(NSL // 4):
            js_ = [j4 * 4 + i for i in range(4) if j4 * 4 + i < 19]
            rrs = {}
            for i, j in enumerate(js_):
                rrs[j] = ln_stats(Xr[:, j, :], ("Xs", j), scr6l[i])
            for i, j in enumerate(js_):
                xrow = Xr[:, j, :]
                scr = scr6l[i]
                P.op("act", lambda e, xrow=xrow, scr=scr: e.activation(out=xrow, in_=xrow, func=AF.Identity, scale=scr[3], bias=scr[4]),
                     reads=[("Xs", j)] + rrs[j], writes=[("Xs", j)])
            for i, j in enumerate(js_):
                xrow = Xr[:, j, :]
                P.op("dve", lambda e, xrow=xrow: e.tensor_tensor(out=xrow, in0=xrow, in1=lnB2[:, 0, :], op=ALU.mult),
                     reads=[("Xs", j), "ln2B0"], writes=[("Xs", j)])
            for i, j in enumerate(js_):
                xrow = Xr[:, j, :]
                P.op("pool", lambda e, xrow=xrow: e.tensor_tensor(out=xrow, in0=xrow, in1=lnB2[:, 1, :], op=ALU.add),
                     reads=[("Xs", j), "ln2B1"], writes=[("Xs", j)])
            for j in js_:
                otoks.append(P.dma("pool", lambda e, j=j: e.indirect_dma_start(
                    out=y_d[:, :], out_offset=bass.IndirectOffsetOnAxis(ap=cws[:, j, 32:33].bitcast(I32), axis=0),
                    in_=Xr[:, j, :], in_offset=None, bounds_check=S - 1, oob_is_err=False),
                    reads=[("Xs", j), "cws"]))
    P.wait_all("sp", otoks)
    P.emit()
    es.close()
    return nc


def _host_tables(rel_bias):
    rel = (np.arange(128)[:, None] - np.arange(384)[None, :] + 128).astype(np.int32)
    half, max_exact, max_dist = 16, 8, 128
    ret = np.where(rel > 0, half, 0)
    n = np.abs(rel)
    nf = np.maximum(n, 1).astype(np.float32)
    lg = np.log(nf / np.float32(max_exact)).astype(np.float32) / np.float32(math.log(max_dist / max_exact)) * np.float32(half - max_exact)
    large = max_exact + lg.astype(np.float32).astype(np.int32)
    large = np.minimum(large, half - 1)
    bucket = ret + np.where(n < max_exact, n, large)
    rb = np.asarray(rel_bias, dtype=np.float32)
    tb = rb[bucket]
    tb = np.ascontiguousarray(tb.transpose(0, 2, 1)).reshape(128, 4 * 384)
    cb = np.zeros((128, 8), np.float32)
    for h in range(4):
        cb[:, 2 * h + 0] = rb[15, h]
        cb[:, 2 * h + 1] = rb[31, h]
    tpos = np.arange(S)
    row = (tpos // 64).astype(np.float32)
    col = (tpos % 64).astype(np.float32)
    freqs = (np.float32(10000.0) ** (-np.arange(0, 32, 2, dtype=np.float32) / np.float32(32))).astype(np.float32)
    ang = np.concatenate([row[:, None] * freqs, col[:, None] * freqs], -1).astype(np.float32)
    cos = np.cos(ang).astype(np.float32)
    sin = np.sin(ang).astype(np.float32)
    C = np.repeat(cos, 2, axis=1)
    Sg = np.stack([-sin, sin], -1).reshape(S, 64)
    C = np.ascontiguousarray(C.reshape(NT, 128, 64).transpose(1, 0, 2)).reshape(128, NT * 64)
    Sg = np.ascontiguousarray(Sg.reshape(NT, 128, 64).transpose(1, 0, 2)).reshape(128, NT * 64)
    return tb.astype(np.float32), cb, C.astype(np.float32), Sg.astype(np.float32)


_CACHE = {}


def kernel(x, c, w_ada, b_ada, w_in, lambda_q1, lambda_k1, lambda_q2, lambda_k2, diff_subln_g, q_norm_g, k_norm_g,
           rel_bias, w_out, ln1_g, ln1_b, w_router_group, b_router_group, w_router_expert, b_router_expert,
           w_gate, w_up, w_down, ln2_g, ln2_b):
    f = lambda a: np.ascontiguousarray(np.asarray(a, dtype=np.float32))
    x = f(x)
    c = f(c)
    tb, cb, rc, rs = _host_tables(rel_bias)
    w_rt = np.concatenate([f(w_router_group)[0], f(w_router_expert)[0].transpose(1, 0, 2).reshape(D, 32)], axis=1)
    b_rt = np.concatenate([f(b_router_group)[0], f(b_router_expert)[0].reshape(32)])
    lam = np.stack([f(lambda_q1)[0], f(lambda_k1)[0], f(lambda_q2)[0], f(lambda_k2)[0]])
    shared = {
        "w_ada": f(w_ada)[0], "b_ada": f(b_ada)[0], "w_in": f(w_in)[0], "lam": f(lam),
        "subg": f(diff_subln_g)[0], "qg": f(q_norm_g)[0], "kg": f(k_norm_g)[0],
        "tbias": tb, "cbias": cb, "ropec": rc, "ropes": rs,
        "w_out": f(w_out)[0], "ln1_g": f(ln1_g)[0], "ln1_b": f(ln1_b)[0],
        "w_rt": f(w_rt), "b_rt": f(b_rt),
        "w_gate": f(w_gate)[0], "w_up": f(w_up)[0], "w_down": f(w_down)[0],
        "ln2_g": f(ln2_g)[0], "ln2_b": f(ln2_b)[0],
    }
    if "nc" not in _CACHE:
        _CACHE["nc"] = build_program()
    nc = _CACHE["nc"]
    in_maps = []
    for b in range(8):
        m = dict(shared)
        m["x"] = x[b]
        m["c"] = c[b]
        in_maps.append(m)
    res = run_bass_kernel_spmd(nc, in_maps, core_ids=list(range(8)))
    out = np.stack([np.asarray(r["y"], dtype=np.float32) for r in res.results], axis=0)
    return out
```

```python
import math
from contextlib import ExitStack

import numpy as np
import concourse.bass as bass
import concourse.mybir as mybir
from concourse.bass_utils import run_bass_kernel_spmd

F32 = mybir.dt.float32
BF16 = mybir.dt.bfloat16
I32 = mybir.dt.int32
AF = mybir.ActivationFunctionType
ALU = mybir.AluOpType
AX = mybir.AxisListType

ENGS = ("pe", "act", "dve", "pool", "sp")
N_DSEM = 8

S = 2048
D = 1024
NT = 16
NE = 32
ALPHA = 2.0 ** 0.25
LAMBDA_INIT = 0.2
LN_EPS = 1e-5
RMS_EPS = 1e-6
N_MOE_EXPERTS = NE
DBG = {}


class Prog:
    def __init__(self, nc):
        self.nc = nc
        self.q = {e: [] for e in ENGS}
        self.seq = {e: 0 for e in ENGS}
        self.seen = {e: {} for e in ENGS}
        self.res = {}
        self.dma_rot = {"sp": 0, "pool": 0}
        self.dma_cnt = {}
        self.skip_ops = None

    def _need(self, eng, tok, waits):
        if tok is None:
            return
        key, val = tok
        if key == eng and eng == "pe":
            return
        cur = self.seen[eng].get(key, 0)
        if val > cur:
            self.seen[eng][key] = val
            waits[key] = max(waits.get(key, 0), val)

    def _deps(self, eng, reads, writes, waits):
        for r in reads:
            st = self.res.get(r)
            if st is not None:
                self._need(eng, st["w"], waits)
                if isinstance(r, tuple) and r[0] == "ps":
                    for t in st["r"]:
                        if t[0] != eng:
                            self._need(eng, t, waits)
        for w in writes:
            st = self.res.get(w)
            if st is not None:
                self._need(eng, st["w"], waits)
                for t in st["r"]:
                    self._need(eng, t, waits)

    def _record(self, tok, reads, writes):
        for r in reads:
            st = self.res.setdefault(r, {"w": None, "r": []})
            st["r"].append(tok)
            if len(st["r"]) > 48:
                best = {}
                for k, v in st["r"]:
                    best[k] = max(best.get(k, 0), v)
                st["r"] = list(best.items())
        for w in writes:
            self.res[w] = {"w": tok, "r": []}

    def alias(self, new, olds):
        toks = []
        for o in olds:
            st = self.res.get(o)
            if st is not None:
                if st["w"] is not None:
                    toks.append(st["w"])
                toks.extend(st["r"])
        self.res[new] = {"w": None, "r": toks}

    def op(self, eng, fn, reads=(), writes=(), inc=True):
        waits = {}
        self._deps(eng, reads, writes, waits)
        if inc:
            self.seq[eng] += 1
            tok = (eng, self.seq[eng])
        else:
            tok = (eng, self.seq[eng] + 1)
        self.q[eng].append(("op", fn, waits, inc))
        self._record(tok, reads, writes)
        return tok

    def dma(self, queue, fn, reads=(), writes=()):
        waits = {}
        self._deps(queue, reads, writes, waits)
        i = self.dma_rot[queue]
        self.dma_rot[queue] = (i + 1) % N_DSEM
        key = ("d", queue, i)
        prev = self.dma_cnt.get(key, 0)
        if prev:
            self._need(queue, (key, prev), waits)
        self.dma_cnt[key] = prev + 16
        tok = (key, prev + 16)
        self.q[queue].append(("dma", fn, waits, key))
        self._record(tok, reads, writes)
        return tok

    def vload(self, name, ap, reads, min_val=0, max_val=64):
        for eng in ("pe", "act", "dve"):
            waits = {}
            self._deps(eng, reads, [], waits)
            self.q[eng].append(("vload", (name, ap, min_val, max_val), waits, None))

    def begin_if(self, cond):
        if not hasattr(self, "_if_stack"):
            self._if_stack = []
        snap = {e: dict(self.seen[e]) for e in ("pe", "act", "dve")}
        self._if_stack.append(({e: self.seq[e] for e in ("pe", "act", "dve")}, snap))
        for eng in ("pe", "act", "dve"):
            self.q[eng].append(("if", cond, {}, None))

    def end_if(self):
        st, snap = self._if_stack.pop()
        if not hasattr(self, "_skip_rot"):
            self._skip_rot = {e: 0 for e in ("pe", "act", "dve")}
            self._skip_last = {e: [0] * 16 for e in ("pe", "act", "dve")}
        for eng in ("pe", "act", "dve"):
            k = self.seq[eng] - st[eng]
            extra = None
            if k > 0:
                slot = self._skip_rot[eng] % 16
                self._skip_rot[eng] += 1
                extra = (slot, min(self._skip_last[eng][slot], st[eng]))
                self._skip_last[eng][slot] = self.seq[eng]
            self.q[eng].append(("else", k, {}, extra))
        for e in ("pe", "act", "dve"):
            self.seen[e] = snap[e]

    def scope(self, name):
        for e in ENGS:
            self.q[e].append(("scope", name, {}, None))

    def barrier(self):
        toks = [(e, self.seq[e]) for e in ("pe", "act", "dve", "pool") if self.seq[e] > 0]
        toks += [(k, v) for k, v in self.dma_cnt.items()]
        for e in ENGS:
            self.wait_all(e, toks)

    def wait_all(self, eng, toks):
        waits = {}
        for t in toks:
            self._need(eng, t, waits)
        self.q[eng].append(("wait", None, waits, None))

    def emit(self):
        nc = self.nc
        with ExitStack() as es:
            sems = {}
            for e in ENGS:
                sems[e] = es.enter_context(nc.semaphore("s_" + e))
            for qn in ("sp", "pool"):
                for i in range(N_DSEM):
                    sems[("d", qn, i)] = es.enter_context(nc.semaphore("d_%s%d" % (qn, i)))
            block = es.enter_context(nc.Block())

            def run(ename):
                def body(eng):
                    vals = {}
                    ctx = []
                    cur_scope = [None]
                    for kind, fn, waits, extra in self.q[ename]:
                        if kind == "scope":
                            if not DBG.get("scopes"):
                                continue
                            if cur_scope[0] is not None:
                                nc.leave_named_scope(cur_scope[0][0], cur_scope[0][1], False)
                            sid, _ = nc.enter_named_scope(fn, False)
                            cur_scope[0] = (fn, sid)
                            continue
                        for k, v in waits.items():
                            eng.wait_ge(sems[k], v)
                        if kind == "vload":
                            name, ap, mn, mx = fn
                            vals[name] = eng.value_load(ap)
                        elif kind == "if":
                            c = eng.If(fn(vals))
                            c.__enter__()
                            ctx.append(c)
                        elif kind == "else":
                            c = ctx.pop()
                            c.__exit__(None, None, None)
                            if fn > 0:
                                c2 = eng.Else()
                                c2.__enter__()
                                if DBG.get("drain_skip") or self.skip_ops is None:
                                    eng.drain()
                                    eng.sem_inc(sems[ename], fn)
                                else:
                                    slot, prev_tok = extra
                                    if prev_tok:
                                        eng.wait_ge(sems[ename], prev_tok)
                                    self.skip_ops[ename](eng, slot).then_inc(sems[ename], fn)
                                c2.__exit__(None, None, None)
                        elif kind == "op":
                            ins = fn(eng)
                            if extra:
                                ins.then_inc(sems[ename], 1)
                        elif kind == "dma":
                            ins = fn(eng)
                            ins.then_inc(sems[extra], 16)
                    if cur_scope[0] is not None:
                        nc.leave_named_scope(cur_scope[0][0], cur_scope[0][1], False)
                return body

            block.tensor(run("pe"))
            block.scalar(run("act"))
            block.vector(run("dve"))
            block.gpsimd(run("pool"))
            block.sync(run("sp"))


class _Stop(Exception):
    pass


def build_program(dbg=None, stop=None):
    nc = bass.Bass("TRN2", target_bir_lowering=False)

    def din(name, shape):
        return nc.dram_tensor(name, list(shape), F32, kind="ExternalInput").ap()

    x_d = din("x", [S, D])
    c_d = din("c", [D])
    w_ada_d = din("w_ada", [D, 6 * D])
    b_ada_d = din("b_ada", [6 * D])
    w_in_d = din("w_in", [D, 2304])
    lam_d = din("lam", [4, 64])
    subg_d = din("subg", [128])
    qg_d = din("qg", [64])
    kg_d = din("kg", [64])
    tb_d = din("tbias", [128, 4 * 384])
    cb_d = din("cbias", [128, 8])
    rc_d = din("ropec", [128, NT * 64])
    rs_d = din("ropes", [128, NT * 64])
    w_out_d = din("w_out", [D, D])
    ln1g_d = din("ln1_g", [D])
    ln1b_d = din("ln1_b", [D])
    wr_d = din("w_rt", [D, 36])
    br_d = din("b_rt", [36])
    wg_d = din("w_gate", [NE, D, 512])
    wu_d = din("w_up", [NE, D, 512])
    wd_d = din("w_down", [NE, 512, D])
    ln2g_d = din("ln2_g", [D])
    ln2b_d = din("ln2_b", [D])
    y_d = nc.dram_tensor("y", [S, D], F32, kind="ExternalOutput").ap()
    Hn_d = nc.dram_tensor("hn_scr", [20 * 128, D], BF16, kind="Internal").ap()
    Xs_d = nc.dram_tensor("xs_scr", [20 * 128, D], F32, kind="Internal").ap()
    Cs_d = nc.dram_tensor("cs_scr", [20 * 128, 48], F32, kind="Internal").ap()
    Ys_d = nc.dram_tensor("ys_scr", [20 * 128, D], F32, kind="Internal").ap()
    dbg_d = None
    if dbg is not None:
        dbg_d = nc.dram_tensor("dbg", list(dbg), F32, kind="ExternalOutput").ap()

    es = ExitStack()
    ARENA_BYTES = 204 * 1024
    arena = es.enter_context(nc.sbuf_tensor("arena", [128, ARENA_BYTES // 4], F32))
    psb = [es.enter_context(nc.psum_tensor("ps%d" % i, [128, 512], F32)) for i in range(8)]

    def view(off, shape, dt):
        n = 1
        for s_ in shape[1:]:
            n *= s_
        nb = n * (2 if dt == BF16 else 4)
        assert off % 4 == 0 and off + nb <= ARENA_BYTES, (off, nb)
        ap = arena[:, off // 4:(off + nb) // 4]
        if dt == BF16:
            ap = ap.bitcast(BF16)
        if len(shape) == 3:
            ap = ap.rearrange("p (a b) -> p a b", b=shape[2])
        elif len(shape) == 4:
            ap = ap.rearrange("p (a b c) -> p a b c", b=shape[2], c=shape[3])
        return ap

    class Alloc:
        def __init__(self, base, limit):
            self.off = base
            self.limit = limit

        def __call__(self, shape, dt):
            n = 1
            for s_ in shape[1:]:
                n *= s_
            nb = n * (2 if dt == BF16 else 4)
            nb = (nb + 63) // 64 * 64
            v = view(self.off, shape, dt)
            self.off += nb
            assert self.off <= self.limit, (self.off, self.limit)
            return v

    KB = 1024
    ca = Alloc(0, 30 * KB)
    ca.limit = 30 * KB
    identf = ca([128, 128], F32)
    ident = ca([128, 128], BF16)
    ones_bf = ca([128, 128], BF16)
    swapM = ca([128, 128], F32)
    Tb = ca([128, 4, 384], F32)
    ropeC = ca([128, NT, 64], F32)
    ropeS = ca([128, NT, 64], F32)
    gB = ca([128, 10, 64], F32)
    g1B = ca([128, D], F32)
    g2B = ca([128, D], F32)
    modT = ca([128, 4, 8], F32)
    cbias = ca([128, 8], F32)
    nlam = ca([128, 1], F32)
    gsub = ca([128, 1], F32)
    epsL = ca([128, 1], F32)
    epsR = ca([128, 1], F32)
    cw = ca([128, NT, 32], F32)
    smallf = ca([128, 256], F32)
    rs2 = ca([128, NT], F32)
    nm2 = ca([128, NT], F32)
    posi = ca([128, NT], F32).bitcast(I32)
    tsi = ca([128, 8], F32).bitcast(I32)
    H_OFF = 30 * KB
    Q_OFF = 70 * KB
    S_OFF = 150 * KB
    hT = view(H_OFF, [128, 8, S], BF16)
    mixT = hT
    NSL = 20
    NSLOT = NSL * 128
    Xr = view(Q_OFF, [128, NSL, D], F32)
    hTs = view(H_OFF, [128, 8, NSLOT], BF16)

    P = Prog(nc)
    ps = [p[:] for p in psb]
    psbf = [p[:].bitcast(BF16) for p in psb]
    skp = ca([128, 16], F32)
    P.skip_ops = {
        "pe": lambda e, i: e.matmul(ps[7][0:1, 2 * i:2 * i + 2], lhsT=ident[0:1, 0:1], rhs=ident[0:1, 0:2], start=True, stop=True),
        "act": lambda e, i: e.activation(out=smallf[0:1, 2 * i:2 * i + 2], in_=skp[0:1, 0:2], func=AF.Silu),
        "dve": lambda e, i: e.tensor_copy(out=smallf[0:1, 32 + 2 * i:34 + 2 * i], in_=skp[0:1, 0:2]),
    }

    def chk(k):
        if stop is not None and stop == k:
            raise _Stop()

    try:
        sa = Alloc(S_OFF, ARENA_BYTES)
        P.op("pool", lambda e: e.memset(identf, 0.0), writes=["identf"])
        P.op("pool", lambda e: e.affine_select(out=identf, in_=identf, pattern=[[-1, 128]], compare_op=ALU.not_equal,
                                               fill=1.0, base=0, channel_multiplier=1), reads=["identf"], writes=["identf"])
        P.op("pool", lambda e: e.memset(swapM, 0.0), writes=["swapM"])
        P.op("pool", lambda e: e.affine_select(out=swapM, in_=swapM, pattern=[[-1, 128]], compare_op=ALU.not_equal,
                                               fill=1.0, base=64, channel_multiplier=1), reads=["swapM"], writes=["swapM"])
        P.op("pool", lambda e: e.affine_select(out=swapM, in_=swapM, pattern=[[-1, 128]], compare_op=ALU.not_equal,
                                               fill=1.0, base=-64, channel_multiplier=1), reads=["swapM"], writes=["swapM"])
        P.op("pool", lambda e: e.memset(ones_bf, 1.0), writes=["ones"])
        P.op("pool", lambda e: e.memset(skp, 0.0), writes=["skp"])
        P.op("pool", lambda e: e.memset(epsL, LN_EPS), writes=["eps"])
        P.op("pool", lambda e: e.memset(epsR, RMS_EPS), writes=["eps"])
        P.op("dve", lambda e: e.tensor_copy(out=ident, in_=identf), reads=["identf"], writes=["ident"])

        P.dma("sp", lambda e: e.dma_start(out=Tb.rearrange("p a b -> p (a b)"), in_=tb_d), writes=["Tb"])
        P.dma("sp", lambda e: e.dma_start(out=cbias, in_=cb_d), writes=["cbias"])
        P.dma("sp", lambda e: e.dma_start(out=ropeC.rearrange("p a b -> p (a b)"), in_=rc_d), writes=["rope"])
        P.dma("sp", lambda e: e.dma_start(out=ropeS.rearrange("p a b -> p (a b)"), in_=rs_d), writes=["rope"])
        lamv = smallf[:, 0:256].rearrange("p (a b) -> p a b", b=64)
        P.dma("sp", lambda e: e.dma_start(out=smallf[:, 0:256], in_=lam_d.rearrange("a b -> (a b)").partition_broadcast(128)),
              writes=["lamv"])
        sc_f = sa([128, 8], F32)
        sc_s = sa([128, 8], F32)
        scB = sa([128, 8, 128], BF16)
        qgk = sa([128, 2, 64], F32)
        lp = sa([128, 2, 64], F32)
        ls = sa([128, 2], F32)
        le = sa([128, 2], F32)
        P.dma("sp", lambda e: e.dma_start(out=sc_f, in_=c_d.rearrange("(c p) -> p c", p=128), allow_slow_non_contiguous=True),
              writes=["sc_f"])
        P.dma("sp", lambda e: e.dma_start(out=qgk[:, 0, :], in_=qg_d.partition_broadcast(128)), writes=["qgk0"])
        P.dma("sp", lambda e: e.dma_start(out=qgk[:, 1, :], in_=kg_d.partition_broadcast(128)), writes=["qgk1"])
        P.dma("sp", lambda e: e.dma_start(out=gsub, in_=subg_d.rearrange("(p o) -> p o", o=1)), writes=["gsub"])
        P.op("dve", lambda e: e.tensor_scalar(out=gB[:, 0:8, :], in0=qgk[:, 0:1, :].to_broadcast([128, 8, 64]), scalar1=0.125,
                                              scalar2=None, op0=ALU.mult), reads=["qgk0"], writes=["gBq"])
        P.op("dve", lambda e: e.tensor_copy(out=gB[:, 8:10, :], in_=qgk[:, 1:2, :].to_broadcast([128, 2, 64])),
             reads=["qgk1"], writes=["gBk"])
        P.op("dve", lambda e: e.tensor_scalar(out=gsub, in0=gsub, scalar1=1.0 - LAMBDA_INIT, scalar2=None, op0=ALU.mult),
             reads=["gsub"], writes=["gsub"])
        P.op("dve", lambda e: e.tensor_tensor(out=lp[:, 0, :], in0=lamv[:, 0, :], in1=lamv[:, 1, :], op=ALU.mult),
             reads=["lamv"], writes=["lp0"])
        P.op("dve", lambda e: e.tensor_tensor(out=lp[:, 1, :], in0=lamv[:, 2, :], in1=lamv[:, 3, :], op=ALU.mult),
             reads=["lamv"], writes=["lp1"])
        P.op("dve", lambda e: e.tensor_reduce(out=ls, in_=lp, axis=AX.X, op=ALU.add), reads=["lp0", "lp1"], writes=["ls"])
        P.op("act", lambda e: e.activation(out=le, in_=ls, func=AF.Exp), reads=["ls"], writes=["le"])
        P.op("dve", lambda e: e.tensor_scalar(out=nlam, in0=le[:, 1:2], scalar1=le[:, 0:1], scalar2=-LAMBDA_INIT,
                                              op0=ALU.subtract, op1=ALU.add), reads=["le"], writes=["nlam"])
        P.op("act", lambda e: e.activation(out=sc_s, in_=sc_f, func=AF.Silu), reads=["sc_f"], writes=["sc_s"])
        P.op("dve", lambda e: e.tensor_copy(out=scB, in_=sc_s.unsqueeze(2).to_broadcast([128, 8, 128])),
             reads=["sc_s"], writes=["scB"])

        chk(-1)
        P.scope("p0_adaln")
        wa = [sa([128, 8, 512], BF16) for _ in range(4)]
        ba = [sa([128, 512], F32) for _ in range(2)]
        modrow = [sa([128, 512], F32) for _ in range(2)]
        for j in range(12):
            b = j % 2
            wb = j % 4
            P.dma("pool", lambda e, j=j, wb=wb: e.dma_start(out=wa[wb], in_=w_ada_d[:, j * 512:(j + 1) * 512].rearrange("(c p) n -> p c n", p=128)),
                  writes=[("wa", wb)])
            P.dma("sp", lambda e, j=j, b=b: e.dma_start(out=ba[b], in_=b_ada_d[j * 512:(j + 1) * 512].partition_broadcast(128)),
                  writes=[("ba", b)])
            pb = j % 2
            for c in range(8):
                P.op("pe", lambda e, c=c, wb=wb, pb=pb: e.matmul(ps[pb], lhsT=scB[:, c, :], rhs=wa[wb][:, c, :], start=(c == 0), stop=(c == 7)),
                     reads=["scB", ("wa", wb)], writes=[("ps", pb)], inc=(c == 7))
            sect = j // 2
            half = j % 2
            if sect in (2, 5):
                dst = (g1B if sect == 2 else g2B)[:, half * 512:(half + 1) * 512]
                P.op("dve", lambda e, dst=dst, b=b, pb=pb: e.scalar_tensor_tensor(out=dst, in0=ps[pb], scalar=1.0, in1=ba[b],
                                                                                 op0=ALU.add, op1=ALU.add),
                     reads=[("ps", pb), ("ba", b)], writes=[("gB", sect, half)])
            else:
                addc = 1.0 if sect in (1, 4) else 0.0
                P.op("dve", lambda e, b=b, pb=pb, addc=addc: e.scalar_tensor_tensor(out=modrow[b], in0=ps[pb], scalar=addc, in1=ba[b],
                                                                                   op0=ALU.add, op1=ALU.add),
                     reads=[("ps", pb), ("ba", b)], writes=[("modrow", b)])
                mi = {0: 0, 1: 1, 3: 2, 4: 3}[sect]
                pc = 2 + (j % 2)
                for i in range(4):
                    P.op("pe", lambda e, i=i, b=b, pc=pc: e.matmul(ps[pc][:, i:i + 1], lhsT=modrow[b][:, i * 128:(i + 1) * 128],
                                                                   rhs=identf[:, 0:1], start=True, stop=True),
                         reads=[("modrow", b), "identf"], writes=[("ps", pc)], inc=(i == 3))
                P.op("dve", lambda e, mi=mi, half=half, pc=pc: e.tensor_copy(out=modT[:, mi, half * 4:(half + 1) * 4], in_=ps[pc][:, 0:4]),
                     reads=[("ps", pc)], writes=[("modT", mi, half)])
        MODT_ALL = [("modT", mi, h_) for mi in range(4) for h_ in range(2)]
        G1B = [("gB", 2, 0), ("gB", 2, 1)]
        G2B = [("gB", 5, 0), ("gB", 5, 1)]

        def ln_stats(src_ap, tag, scr):
            st, mv, lnv, rstd, nmr = scr
            for jj in range(2):
                P.op("dve", lambda e, jj=jj: e.bn_stats(out=st[:, jj, :], in_=src_ap[:, jj * 512:(jj + 1) * 512]),
                     reads=[tag], writes=[("st", id(st), jj)])
            P.op("dve", lambda e: e.bn_aggr(out=mv, in_=st.rearrange("p a b -> p (a b)")),
                 reads=[("st", id(st), 0), ("st", id(st), 1)], writes=[("mv", id(mv))])
            P.op("act", lambda e: e.activation(out=lnv, in_=mv[:, 1:2], func=AF.Ln, bias=epsL, scale=1.0),
                 reads=[("mv", id(mv)), "eps"], writes=[("lnv", id(lnv))])
            P.op("act", lambda e: e.activation(out=rstd, in_=lnv, func=AF.Exp, scale=-0.5),
                 reads=[("lnv", id(lnv))], writes=[("rstd", id(rstd))])
            P.op("dve", lambda e: e.scalar_tensor_tensor(out=nmr, in0=mv[:, 0:1], scalar=-1.0, in1=rstd, op0=ALU.mult, op1=ALU.mult),
                 reads=[("mv", id(mv)), ("rstd", id(rstd))], writes=[("nmr", id(nmr))])
            return [("rstd", id(rstd)), ("nmr", id(nmr))]

        def ln_scr(al):
            return (al([128, 2, 6], F32), al([128, 2], F32), al([128, 1], F32), al([128, 1], F32), al([128, 1], F32))

        def to_hT(xn_ap, xn_res, g4, sect_scale, sect_bias, psrot, hT=hT, hres="hT"):
            for c in range(8):
                pb = psrot[c % len(psrot)]
                for t in range(4):
                    P.op("pe", lambda e, c=c, t=t, pb=pb: e.transpose(out=psbf[pb][:, t * 128:(t + 1) * 128],
                                                                     in_=xn_ap[:, t, c * 128:(c + 1) * 128], identity=ident),
                         reads=[xn_res, "ident"], writes=[("ps", pb)], inc=(t == 3))
                eng = "act" if c % 2 == 0 else "dve"
                if eng == "act":
                    P.op("act", lambda e, c=c, pb=pb: e.activation(out=hT[:, c, g4 * 512:(g4 + 1) * 512], in_=psbf[pb][:, 0:512],
                                                                   func=AF.Identity, scale=modT[:, sect_scale, c:c + 1],
                                                                   bias=modT[:, sect_bias, c:c + 1]),
                         reads=[("ps", pb)] + MODT_ALL, writes=[(hres, g4)])
                else:
                    P.op("dve", lambda e, c=c, pb=pb: e.tensor_scalar(out=hT[:, c, g4 * 512:(g4 + 1) * 512], in0=psbf[pb][:, 0:512],
                                                                      scalar1=modT[:, sect_scale, c:c + 1],
                                                                      scalar2=modT[:, sect_bias, c:c + 1], op0=ALU.mult, op1=ALU.add),
                         reads=[("ps", pb)] + MODT_ALL, writes=[(hres, g4)])

        chk(0)
        P.scope("p1_ln")
        sa1 = Alloc(S_OFF + 51 * KB, ARENA_BYTES)
        xt = [view(Q_OFF + i * 16 * KB, [128, 4, D], F32) for i in range(2)]
        xn = [view(Q_OFF + 32 * KB + i * 8 * KB, [128, 4, D], BF16) for i in range(2)]
        scr1 = [ln_scr(sa1) for _ in range(2)]
        for g4 in range(4):
            b = g4 % 2
            P.dma("sp", lambda e, g4=g4, b=b: e.dma_start(out=xt[b], in_=x_d[g4 * 512:(g4 + 1) * 512, :].rearrange("(t p) d -> p t d", p=128)),
                  writes=[("xt", b)])
            for t in range(4):
                scr = scr1[t % 2]
                rr = ln_stats(xt[b][:, t, :], ("xt", b), scr)
                P.op("act", lambda e, b=b, t=t, scr=scr: e.activation(out=xn[b][:, t, :], in_=xt[b][:, t, :], func=AF.Identity,
                                                                      scale=scr[3], bias=scr[4]),
                     reads=[("xt", b)] + rr, writes=[("xn", b)])
            to_hT(xn[b], ("xn", b), g4, 1, 0, [4, 5, 6, 7])

        chk(1)
        P.scope("p2_inproj")
        dqT = view(Q_OFF, [128, 4, S], BF16)
        dkT = view(Q_OFF + 16 * KB, [128, 4, S], BF16)
        dv = view(Q_OFF + 32 * KB, [128, NT, 512], BF16)
        gqT = view(Q_OFF + 48 * KB, [128, 4, S], BF16)
        gkd = view(Q_OFF + 64 * KB, [128, 2, S], BF16)
        gv = view(Q_OFF + 72 * KB, [128, NT, 2, 128], BF16)
        P.barrier()
        sa2 = Alloc(S_OFF, ARENA_BYTES)
        wi = [sa2([128, 8, 512], BF16) for _ in range(4)] + [sa2([128, 8, 256], BF16)]
        t_sq = sa2([128, 8, 64], F32)
        t_xn = sa2([128, 8, 64], F32)
        t_t1 = sa2([128, 8, 64], F32)
        t_t2 = sa2([128, 8, 64], F32)
        t_ss = sa2([128, 8], F32)
        t_ln = sa2([128, 8], F32)
        t_rs = sa2([128, 8], F32)
        qrope = sa2([128, 4, 512], BF16)
        krope = sa2([128, 4, 256], BF16)

        def load_wi(ci, c0, ncol):
            b = ci
            P.dma("pool", lambda e: e.dma_start(out=wi[b][:, :, 0:ncol], in_=w_in_d[:, c0:c0 + ncol].rearrange("(c p) n -> p c n", p=128)),
                  writes=[("wi", b)])
            return b

        P.op("pool", lambda e: e.memset(gv[:, :, :, 64:128], 1.0), reads=[], writes=["gv_ones"])
        HT_ALL = [("hT", g) for g in range(4)]
        WB = {}
        WB[3] = load_wi(3, 1536, 512)
        WB[4] = load_wi(4, 2048, 256)
        WB[0] = load_wi(0, 0, 512)
        WB[1] = load_wi(1, 512, 512)
        WB[2] = load_wi(2, 1024, 512)
        heavy = []
        hv_rot = {"i": 0}

        def hv_bank():
            hv_rot["i"] += 1
            return 4 + hv_rot["i"] % 2

        for ci in range(2):
            b = WB[ci]
            dst = dqT if ci == 0 else dkT
            for blk in range(4):
                for tt in range(4):
                    def unit(ci=ci, b=b, blk=blk, tt=tt):
                        pb = hv_bank()
                        for c in range(8):
                            P.op("pe", lambda e, c=c, b=b, blk=blk, tt=tt, pb=pb: e.matmul(ps[pb], lhsT=wi[b][:, c, blk * 128:(blk + 1) * 128],
                                                                                         rhs=hT[:, c, tt * 512:(tt + 1) * 512],
                                                                                         start=(c == 0), stop=(c == 7)),
                                 reads=[("wi", b), ("hT", tt)], writes=[("ps", pb)], inc=(c == 7))
                        if ci == 0:
                            P.op("act", lambda e, blk=blk, tt=tt, pb=pb: e.activation(out=dqT[:, blk, tt * 512:(tt + 1) * 512], in_=ps[pb],
                                                                                     func=AF.Copy, scale=0.125),
                                 reads=[("ps", pb)], writes=[("dqT", blk)])
                        else:
                            P.op("act", lambda e, blk=blk, tt=tt, pb=pb: e.activation(out=dkT[:, blk, tt * 512:(tt + 1) * 512], in_=ps[pb],
                                                                                     func=AF.Copy),
                                 reads=[("ps", pb)], writes=[("dkT", blk)])
                    heavy.append(unit)
        b = WB[2]
        for t in range(NT):
            def unit(b=b, t=t):
                pb = hv_bank()
                for c in range(8):
                    P.op("pe", lambda e, c=c, b=b, t=t, pb=pb: e.matmul(ps[pb], lhsT=hT[:, c, t * 128:(t + 1) * 128], rhs=wi[b][:, c, :],
                                                                      start=(c == 0), stop=(c == 7)),
                         reads=[("wi", b), ("hT", t // 4)], writes=[("ps", pb)], inc=(c == 7))
                P.op("act", lambda e, t=t, pb=pb: e.activation(out=dv[:, t, :], in_=ps[pb], func=AF.Copy),
                     reads=[("ps", pb)], writes=["dv"])
            heavy.append(unit)

        def rms_rope(src, nh, t, gslice, out_writes):
            s3 = src.rearrange("p (h d) -> p h d", d=64)
            sq, xn_, t1, t2 = t_sq[:, 0:nh, :], t_xn[:, 0:nh, :], t_t1[:, 0:nh, :], t_t2[:, 0:nh, :]
            ss, ln_, rs = t_ss[:, 0:nh], t_ln[:, 0:nh], t_rs[:, 0:nh]
            srcres = out_writes[0][2]
            P.op("act", lambda e: e.activation(out=sq, in_=s3, func=AF.Square), reads=[srcres], writes=["t_sq"])
            P.op("dve", lambda e: e.tensor_reduce(out=ss, in_=sq, axis=AX.X, op=ALU.add), reads=["t_sq"], writes=["t_ss"])
            P.op("act", lambda e: e.activation(out=ln_, in_=ss, func=AF.Ln, bias=epsR, scale=1.0 / 64), reads=["t_ss", "eps"], writes=["t_ln"])
            P.op("act", lambda e: e.activation(out=rs, in_=ln_, func=AF.Exp, scale=-0.5), reads=["t_ln"], writes=["t_rs"])
            P.op("dve", lambda e: e.tensor_tensor(out=xn_, in0=s3, in1=rs.unsqueeze(2).to_broadcast([128, nh, 64]), op=ALU.mult),
                 reads=[srcres, "t_rs"], writes=["t_xn"])
            P.op("dve", lambda e: e.tensor_tensor(out=xn_, in0=xn_, in1=gslice, op=ALU.mult), reads=["t_xn", "gBq", "gBk"], writes=["t_xn"])
            cB = ropeC[:, t:t + 1, :].to_broadcast([128, nh, 64])
            P.op("dve", lambda e: e.tensor_tensor(out=t1, in0=xn_, in1=cB, op=ALU.mult), reads=["t_xn", "rope"], writes=["t_t1"])
            xn4 = xn_.rearrange("p h (i two) -> p h i two", two=2)
            t24 = t2.rearrange("p h (i two) -> p h i two", two=2)
            s4 = ropeS[:, t, :].rearrange("p (i two) -> p i two", two=2)
            P.op("dve", lambda e: e.tensor_tensor(out=t24[:, :, :, 0], in0=xn4[:, :, :, 1],
                                                  in1=s4[:, :, 0].unsqueeze(1).to_broadcast([128, nh, 32]), op=ALU.mult),
                 reads=["t_xn", "rope"], writes=["t_t2a"])
            P.op("dve", lambda e: e.tensor_tensor(out=t24[:, :, :, 1], in0=xn4[:, :, :, 0],
                                                  in1=s4[:, :, 1].unsqueeze(1).to_broadcast([128, nh, 32]), op=ALU.mult),
                 reads=["t_xn", "rope"], writes=["t_t2b"])
            for (oap, ores, _) in out_writes:
                P.op("dve", lambda e, oap=oap: e.tensor_tensor(out=oap, in0=t1, in1=t2, op=ALU.add),
                     reads=["t_t1", "t_t2a", "t_t2b"], writes=[ores])

        b3 = WB[3]
        b4 = WB[4]
        for g4 in range(4):
            for tl in range(4):
                t = g4 * 4 + tl
                pq = 0 + (t % 2)
                pk = 2 + (t % 2)
                for c in range(8):
                    P.op("pe", lambda e, c=c, t=t, pq=pq: e.matmul(ps[pq], lhsT=hT[:, c, t * 128:(t + 1) * 128], rhs=wi[b3][:, c, :],
                                                                  start=(c == 0), stop=(c == 7)),
                         reads=[("wi", b3), ("hT", g4)], writes=[("ps", pq)], inc=(c == 7))
                for c in range(8):
                    P.op("pe", lambda e, c=c, t=t, pk=pk: e.matmul(ps[pk][:, 0:256], lhsT=hT[:, c, t * 128:(t + 1) * 128], rhs=wi[b4][:, c, 0:256],
                                                                  start=(c == 0), stop=(c == 7)),
                         reads=[("wi", b4), ("hT", g4)], writes=[("ps", pk)], inc=(c == 7))
                P.op("act", lambda e, t=t, pk=pk: e.activation(out=gv[:, t, :, 0:64], in_=ps[pk][:, 128:256].rearrange("p (g d) -> p g d", d=64),
                                                              func=AF.Copy),
                     reads=[("ps", pk)], writes=["gv"])
                rms_rope(ps[pq], 8, t, gB[:, 0:8, :],
                         [(qrope[:, tl, :].rearrange("p (h d) -> p h d", d=64), ("qrope", tl), ("ps", pq))])
                k3 = krope[:, tl, :].rearrange("p (h d) -> p h d", d=64)
                rms_rope(ps[pk][:, 0:128], 2, t, gB[:, 8:10, :],
                         [(k3[:, 0:2, :], ("kropeA", tl), ("ps", pk))])
                P.op("dve", lambda e, k3=k3: e.tensor_copy(out=k3[:, 2, :], in_=k3[:, 1, :]), reads=[("kropeA", tl)],
                     writes=[("kropeB", tl)])
                P.op("dve", lambda e, k3=k3: e.tensor_copy(out=k3[:, 3, :], in_=k3[:, 0, :]), reads=[("kropeA", tl), ("kropeB", tl)],
                     writes=[("kropeB", tl)])
                for _ in range(3):
                    if heavy:
                        heavy.pop(0)()
            QR = [("qrope", tl) for tl in range(4)]
            KR = [("kropeA", tl) for tl in range(4)] + [("kropeB", tl) for tl in range(4)]
            for pair in range(4):
                pb = 6 + pair % 2
                for tl in range(4):
                    P.op("pe", lambda e, pair=pair, tl=tl, pb=pb: e.transpose(out=psbf[pb][:, tl * 128:(tl + 1) * 128],
                                                                             in_=qrope[:, tl, pair * 128:(pair + 1) * 128], identity=ident),
                         reads=QR + ["ident"], writes=[("ps", pb)], inc=(tl == 3))
                if pair % 2 == 0:
                    P.op("act", lambda e, pair=pair, pb=pb, g4=g4: e.activation(out=gqT[:, pair, g4 * 512:(g4 + 1) * 512], in_=psbf[pb][:, 0:512], func=AF.Copy),
                         reads=[("ps", pb)], writes=[("gqT", pair)])
                else:
                    P.op("dve", lambda e, pair=pair, pb=pb, g4=g4: e.tensor_copy(out=gqT[:, pair, g4 * 512:(g4 + 1) * 512], in_=psbf[pb][:, 0:512]),
                         reads=[("ps", pb)], writes=[("gqT", pair)])
            for ab in range(2):
                pb = 6 + ab
                for tl in range(4):
                    P.op("pe", lambda e, ab=ab, tl=tl, pb=pb: e.transpose(out=psbf[pb][:, tl * 128:(tl + 1) * 128],
                                                                         in_=krope[:, tl, ab * 128:(ab + 1) * 128], identity=ident),
                         reads=KR + ["ident"], writes=[("ps", pb)], inc=(tl == 3))
            sl = slice(g4 * 512, (g4 + 1) * 512)
            P.op("dve", lambda e, sl=sl: e.tensor_copy(out=gkd[0:64, 0, sl], in_=psbf[6][0:64, 0:512]), reads=[("ps", 6)], writes=[("gkd", 0)])
            P.op("dve", lambda e, sl=sl: e.tensor_copy(out=gkd[64:128, 1, sl], in_=psbf[6][64:128, 0:512]), reads=[("ps", 6)], writes=[("gkd", 1)])
            P.op("act", lambda e, sl=sl: e.activation(out=gkd[64:128, 0, sl], in_=psbf[7][64:128, 0:512], func=AF.Copy), reads=[("ps", 7)], writes=[("gkd", 0)])
            P.op("act", lambda e, sl=sl: e.activation(out=gkd[0:64, 1, sl], in_=psbf[7][0:64, 0:512], func=AF.Copy), reads=[("ps", 7)], writes=[("gkd", 1)])

        while heavy:
            heavy.pop(0)()
        chk(2)
        P.scope("p3_diff")
        P.barrier()
        sa3 = Alloc(S_OFF, ARENA_BYTES)
        NPT = 6
        pT = [sa3([128, 512], BF16) for _ in range(NPT)]
        btmp = [sa3([128, 384], F32) for _ in range(2)]
        e_r0 = sa3([128, 512], F32)
        e_r1 = sa3([128, 512], F32)
        e_a = sa3([128, 512], F32)
        e_b = sa3([128, 512], F32)
        e_sq = sa3([128, 512], BF16)
        e_ln = sa3([128, 512], F32)
        first_use = {"v": True}
        ztile = sa3([128, D], F32)
        P.op("pool", lambda e: e.memset(ztile, 0.0), writes=["ztile"])
        P.dma("sp", lambda e: e.dma_start(out=Xs_d.rearrange("(j p) d -> p j d", p=128), in_=ztile.unsqueeze(1).to_broadcast([128, NSL, D])),
              reads=["ztile"], writes=["zf_xs"])
        P.dma("sp", lambda e: e.dma_start(out=Hn_d.rearrange("(j p) d -> p j d", p=128),
                                          in_=ztile.bitcast(BF16)[:, 0:D].unsqueeze(1).to_broadcast([128, NSL, D])),
              reads=["ztile"], writes=["zf_hn"])
        zcw = sa3([128, 48], F32)
        P.op("pool", lambda e: e.memset(zcw[:, 0:32], 0.0), writes=["zcw"])
        P.op("pool", lambda e: e.memset(zcw[:, 32:48].bitcast(I32), 1 << 20), reads=["zcw"], writes=["zcw"])
        P.dma("sp", lambda e: e.dma_start(out=Cs_d.rearrange("(j p) d -> p j d", p=128), in_=zcw.unsqueeze(1).to_broadcast([128, NSL, 48])),
              reads=["zcw"], writes=["zf_cs"])

        def s3w(name):
            return [name]

        pt_rot = {"i": 0}
        bt_rot = {"i": 0}

        def exp_tile(h, c, kt, qt, sbank, diff):
            bi = pt_rot["i"] % NPT
            pt_rot["i"] += 1
            segs = []
            if diff and not DBG.get("nonear"):
                cur = None
                for i in range(4):
                    qs = 4 * qt + i
                    ty = "L" if qs < kt - 1 else ("R" if qs > kt + 1 else "N")
                    if DBG.get("nolr") and ty != "N":
                        ty = "Z"
                    if DBG.get("non") and ty == "N":
                        ty = "Z"
                    if cur is not None and cur[0] == ty:
                        cur[2] = i + 1
                    else:
                        cur = [ty, i, i + 1]
                        segs.append(cur)
            else:
                segs = [["Z", 0, 4]]
            for ty, a, b_ in segs:
                cs = slice(a * 128, b_ * 128)
                if ty == "N":
                    w = (b_ - a) * 128
                    off = (4 * qt + a - kt + 1) * 128
                    bt = bt_rot["i"] % 2
                    bt_rot["i"] += 1
                    P.op("dve", lambda e, cs=cs, w=w, off=off, bt=bt: e.tensor_tensor(out=btmp[bt][:, 0:w], in0=ps[sbank][:, cs],
                                                                                     in1=Tb[:, h, off:off + w], op=ALU.add),
                         reads=[("ps", sbank), "Tb"], writes=s3w(("btmp", bt)))
                    P.op("act", lambda e, cs=cs, w=w, bt=bt: e.activation(out=pT[bi][:, cs], in_=btmp[bt][:, 0:w], func=AF.Exp),
                         reads=[("btmp", bt)], writes=s3w(("pT", bi)))
                elif ty == "Z":
                    P.op("act", lambda e, cs=cs: e.activation(out=pT[bi][:, cs], in_=ps[sbank][:, cs], func=AF.Exp),
                         reads=[("ps", sbank)], writes=s3w(("pT", bi)))
                else:
                    col = 2 * h + (1 if ty == "L" else 0)
                    P.op("act", lambda e, cs=cs, col=col: e.activation(out=pT[bi][:, cs], in_=ps[sbank][:, cs], func=AF.Exp,
                                                                      bias=cbias[:, col:col + 1]),
                         reads=[("ps", sbank), "cbias"], writes=s3w(("pT", bi)))
            return bi

        def diff_qk(h, qt, kt, par):
            for c in range(2):
                sb_ = 2 * par + c
                P.op("pe", lambda e, c=c, sb_=sb_: e.matmul(ps[sb_], lhsT=dkT[c * 64:(c + 1) * 64, h, kt * 128:(kt + 1) * 128],
                                                            rhs=dqT[c * 64:(c + 1) * 64, h, qt * 512:(qt + 1) * 512], start=True, stop=True),
                     reads=[("dqT", h), ("dkT", h)], writes=[("ps", sb_)], inc=True)

        def diff_pv(h, qt, kt, bis):
            for c in range(2):
                P.op("pe", lambda e, c=c: e.matmul(ps[4 + c], lhsT=dv[:, kt, h * 128:(h + 1) * 128], rhs=pT[bis[c]],
                                                   start=(kt == 0), stop=(kt == NT - 1)),
                     reads=["dv", ("pT", bis[c])], writes=[("ps", 4 + c)], inc=(kt == NT - 1))
                P.op("pe", lambda e, c=c: e.matmul(ps[6 + c], lhsT=ones_bf, rhs=pT[bis[c]], start=(kt == 0), stop=(kt == NT - 1)),
                     reads=["ones", ("pT", bis[c])], writes=[("ps", 6 + c)], inc=True)

        def diff_epi_head(h, qt):
            P.op("act", lambda e: e.activation(out=e_r0, in_=ps[6], func=AF.Ln), reads=[("ps", 6)], writes=s3w("e_r0"))
            P.op("act", lambda e: e.activation(out=e_r0, in_=e_r0, func=AF.Exp, scale=-1.0), reads=["e_r0"], writes=["e_r0"])
            P.op("dve", lambda e: e.tensor_tensor(out=e_a, in0=ps[4], in1=e_r0, op=ALU.mult), reads=[("ps", 4), "e_r0"], writes=s3w("e_a"))
            P.op("act", lambda e: e.activation(out=e_r1, in_=ps[7], func=AF.Ln), reads=[("ps", 7)], writes=s3w("e_r1"))
            P.op("act", lambda e: e.activation(out=e_r1, in_=e_r1, func=AF.Exp, scale=-1.0), reads=["e_r1"], writes=["e_r1"])
            P.op("dve", lambda e: e.tensor_tensor(out=e_b, in0=ps[5], in1=e_r1, op=ALU.mult), reads=[("ps", 5), "e_r1"], writes=s3w("e_b"))
            P.op("dve", lambda e: e.scalar_tensor_tensor(out=e_a, in0=e_b, scalar=nlam, in1=e_a, op0=ALU.mult, op1=ALU.add),
                 reads=["e_a", "e_b", "nlam"], writes=["e_a"])

        def diff_epi_tail(h, qt, bank):
            qs = slice(qt * 512, (qt + 1) * 512)
            P.op("act", lambda e: e.activation(out=e_sq, in_=e_a, func=AF.Square), reads=["e_a"], writes=s3w("e_sq"))
            P.op("pe", lambda e: e.matmul(ps[bank], lhsT=ones_bf, rhs=e_sq, start=True, stop=True), reads=["ones", "e_sq"], writes=[("ps", bank)])
            P.op("act", lambda e: e.activation(out=e_ln, in_=ps[bank], func=AF.Ln, bias=epsR, scale=1.0 / 128), reads=[("ps", bank), "eps"],
                 writes=s3w("e_ln"))
            P.op("act", lambda e: e.activation(out=e_ln, in_=e_ln, func=AF.Exp, scale=-0.5), reads=["e_ln"], writes=["e_ln"])
            P.op("dve", lambda e: e.scalar_tensor_tensor(out=mixT[:, h, qs], in0=e_a, scalar=gsub, in1=e_ln, op0=ALU.mult, op1=ALU.mult),
                 reads=["e_a", "e_ln", "gsub"], writes=[("hT", qt)])

        items = [(h, qt, kt) for h in range(4) for qt in range(4) for kt in range(NT)]
        items = items[:DBG.get("diff_n", len(items))]
        pend_tail = None
        for idx, (h, qt, kt) in enumerate(items):
            if idx == 0:
                diff_qk(h, qt, kt, 0)
            if idx + 1 < len(items):
                h2, qt2, kt2 = items[idx + 1]
                diff_qk(h2, qt2, kt2, (idx + 1) % 2)
            par = idx % 2
            bis = [exp_tile(h, c, kt, qt, 2 * par + c, True) for c in range(2)]
            if not DBG.get("nopv"):
                diff_pv(h, qt, kt, bis)
            if pend_tail is not None and idx >= pend_tail[0]:
                diff_epi_tail(pend_tail[1], pend_tail[2], 2 * par)
                pend_tail = None
            if kt == NT - 1 and not DBG.get("noepi"):
                diff_epi_head(h, qt)
                pend_tail = (idx + 2, h, qt)
                first_use["v"] = False
        if pend_tail is not None:
            diff_epi_tail(pend_tail[1], pend_tail[2], 0)
            pend_tail = None

        chk(2.5)
        P.scope("p3_gqa")
        g_rt = e_r0
        g_ot = e_a

        def gqa_qk(pair, qt, kt, par):
            g = pair // 2
            for c in range(2):
                sb_ = 2 * par + c
                P.op("pe", lambda e, c=c, sb_=sb_: e.matmul(ps[sb_], lhsT=gkd[c * 64:(c + 1) * 64, g, kt * 128:(kt + 1) * 128],
                                                            rhs=gqT[c * 64:(c + 1) * 64, pair, qt * 512:(qt + 1) * 512], start=True, stop=True),
                     reads=[("gqT", pair), ("gkd", g)], writes=[("ps", sb_)], inc=True)

        def gqa_pv(pair, qt, kt, bis):
            g = pair // 2
            for c in range(2):
                P.op("pe", lambda e, c=c: e.matmul(ps[4 + c], lhsT=gvl[c][:, kt, g, :], rhs=pT[bis[c]], start=(kt == 0), stop=(kt == NT - 1)),
                     reads=["gv", "gv_ones", "gvB", ("pT", bis[c])], writes=[("ps", 4 + c)], inc=(c == 1 or kt == NT - 1))

        gvB = view(Q_OFF, [128, NT, 2, 128], BF16)
        P.alias("GVB", [("dqT", i) for i in range(4)])
        P.op("dve", lambda e: e.tensor_copy(out=gvB[:, :, :, 64:128], in_=gv[:, :, :, 0:64]), reads=["gv"], writes=["GVB", "gvB"])
        P.op("dve", lambda e: e.tensor_copy(out=gvB[:, :, :, 0:64], in_=gv[:, :, :, 64:128]), reads=["gv_ones", "gvB"], writes=["gvB"])
        gvl = [gv, gvB]

        def gqa_epi_head(pair, qt):
            P.op("act", lambda e: e.activation(out=g_rt[64:128, :], in_=ps[4][64:128, :], func=AF.Ln), reads=[("ps", 4)], writes=["e_r0"])
            P.op("dve", lambda e: e.tensor_copy(out=g_ot[0:64, :], in_=ps[4][0:64, :]), reads=[("ps", 4)], writes=["e_a"])
            P.op("act", lambda e: e.activation(out=g_rt[0:64, :], in_=ps[5][0:64, :], func=AF.Ln), reads=[("ps", 5)], writes=["e_r0"])
            P.op("dve", lambda e: e.tensor_copy(out=g_ot[64:128, :], in_=ps[5][64:128, :]), reads=[("ps", 5)], writes=["e_a"])
            P.op("act", lambda e: e.activation(out=g_rt, in_=g_rt, func=AF.Exp, scale=-1.0), reads=["e_r0"], writes=["e_r0"])

        def gqa_epi_tail(pair, qt, bank):
            qs = slice(qt * 512, (qt + 1) * 512)
            P.op("pe", lambda e: e.matmul(ps[bank], lhsT=swapM, rhs=g_rt, start=True, stop=True), reads=["swapM", "e_r0"], writes=[("ps", bank)])
            P.op("dve", lambda e: e.tensor_tensor(out=mixT[:, 4 + pair, qs], in0=g_ot, in1=ps[bank], op=ALU.mult),
                 reads=["e_a", ("ps", bank)], writes=[("hT", qt)])

        items = [(pair, qt, kt) for pair in range(4) for qt in range(4) for kt in range(NT)]
        pend_tail = None
        n_epi = 0
        for idx, (pair, qt, kt) in enumerate(items):
            if idx == 0:
                gqa_qk(pair, qt, kt, 0)
            if idx + 1 < len(items):
                p2, qt2, kt2 = items[idx + 1]
                gqa_qk(p2, qt2, kt2, (idx + 1) % 2)
            par = idx % 2
            bis = [exp_tile(0, c, kt, qt, 2 * par + c, False) for c in range(2)]
            gqa_pv(pair, qt, kt, bis)
            if pend_tail is not None and idx >= pend_tail[0]:
                gqa_epi_tail(pend_tail[1], pend_tail[2], 6 + (pend_tail[3] % 2))
                pend_tail = None
            if kt == NT - 1:
                gqa_epi_head(pair, qt)
                pend_tail = (idx + 2, pair, qt, n_epi)
                n_epi += 1
        if pend_tail is not None:
            gqa_epi_tail(pend_tail[1], pend_tail[2], 6)
            pend_tail = None

        chk(3)
        P.scope("p4_outproj_ln1")
        P.barrier()
        sa4 = Alloc(S_OFF, ARENA_BYTES)
        wo = [sa4([128, 8, 512], BF16) for _ in range(2)]
        xre = [sa4([128, D], F32) for _ in range(4)]
        lnB = sa4([128, 2, D], F32)
        xn2 = sa4([128, 4, D], BF16)
        scr4 = [ln_scr(sa4) for _ in range(4)]
        scr4b = [ln_scr(sa4) for _ in range(4)]
        first4 = {"v": True}

        def s4w(names):
            return list(names) + (["S4"])

        for hf in range(2):
            P.dma("pool", lambda e, hf=hf: e.dma_start(out=wo[hf], in_=w_out_d[:, hf * 512:(hf + 1) * 512].rearrange("(c p) n -> p c n", p=128)),
                  writes=[("wo", hf)])
        P.dma("sp", lambda e: e.dma_start(out=lnB[:, 0, :], in_=ln1g_d.partition_broadcast(128)), writes=["lnB0"])
        P.dma("sp", lambda e: e.dma_start(out=lnB[:, 1, :], in_=ln1b_d.partition_broadcast(128)), writes=["lnB1"])

        keep = []
        STAT2 = {}
        for g4 in range(4):
            tiles = [g4 * 4 + tl for tl in range(4)]
            for tl, t in enumerate(tiles):
                xb = tl
                P.dma("sp", lambda e, t=t, xb=xb: e.dma_start(out=xre[xb], in_=x_d[t * 128:(t + 1) * 128, :]), writes=[("xre", xb)])
            for tl, t in enumerate(tiles):
                xb = tl
                for hf in range(2):
                    pb = 2 * (t % 2) + hf
                    for c in range(8):
                        P.op("pe", lambda e, c=c, t=t, hf=hf, pb=pb: e.matmul(ps[pb], lhsT=mixT[:, c, t * 128:(t + 1) * 128], rhs=wo[hf][:, c, :],
                                                                            start=(c == 0), stop=(c == 7)),
                             reads=[("hT", g4), ("wo", hf)], writes=[("ps", pb)], inc=(c == 7))
                    P.op("dve", lambda e, hf=hf, pb=pb, t=t: e.tensor_tensor(out=Xr[:, t, hf * 512:(hf + 1) * 512], in0=ps[pb],
                                                                            in1=g1B[:, hf * 512:(hf + 1) * 512], op=ALU.mult),
                         reads=[("ps", pb)] + G1B, writes=[("X", t)])
                xrow = Xr[:, t, :]
                P.op("dve", lambda e, xb=xb, xrow=xrow: e.scalar_tensor_tensor(out=xrow, in0=xre[xb], scalar=ALPHA, in1=xrow,
                                                                              op0=ALU.mult, op1=ALU.add),
                     reads=[("xre", xb), ("X", t)], writes=[("X", t)])
            rr1 = {}
            for tl, t in enumerate(tiles):
                rr1[t] = ln_stats(Xr[:, t, :], ("X", t), scr4[tl])
            for tl, t in enumerate(tiles):
                xrow = Xr[:, t, :]
                scr = scr4[tl]
                P.op("act", lambda e, xrow=xrow, scr=scr: e.activation(out=xrow, in_=xrow, func=AF.Identity, scale=scr[3], bias=scr[4]),
                     reads=[("X", t)] + rr1[t], writes=[("X", t)])
            for tl, t in enumerate(tiles):
                xrow = Xr[:, t, :]
                P.op("dve", lambda e, xrow=xrow: e.tensor_tensor(out=xrow, in0=xrow, in1=lnB[:, 0, :], op=ALU.mult),
                     reads=[("X", t), "lnB0"], writes=[("X", t)])
            for tl, t in enumerate(tiles):
                xrow = Xr[:, t, :]
                P.op("dve", lambda e, xrow=xrow: e.tensor_tensor(out=xrow, in0=xrow, in1=lnB[:, 1, :], op=ALU.add),
                     reads=[("X", t), "lnB1"], writes=[("X", t)])
            for tl, t in enumerate(tiles):
                sb_ = scr4b[tl]
                scr = (sb_[0], sb_[1], sb_[2], rs2[:, t:t + 1], nm2[:, t:t + 1])
                keep.append(scr)
                STAT2[t] = ln_stats(Xr[:, t, :], ("X", t), scr)
            for tl, t in enumerate(tiles):
                xrow = Xr[:, t, :]
                P.op("act", lambda e, xrow=xrow, t=t, tl=tl: e.activation(out=xn2[:, tl, :], in_=xrow, func=AF.Identity, scale=rs2[:, t:t + 1],
                                                                         bias=nm2[:, t:t + 1]),
                     reads=[("X", t)] + STAT2[t], writes=["xn2"])
            to_hT(xn2, "xn2", g4, 3, 2, [4, 5, 6, 7])

        chk(3.5)
        P.scope("p4b_router_sort")
        P.barrier()
        sa4 = Alloc(S_OFF, ARENA_BYTES)
        wr = sa4([128, 8, 36], BF16)
        brB = sa4([128, 36], F32)
        P.dma("pool", lambda e: e.dma_start(out=wr, in_=wr_d.rearrange("(c p) n -> p c n", p=128)), writes=["wr"])
        P.dma("sp", lambda e: e.dma_start(out=brB, in_=br_d.partition_broadcast(128)), writes=["brB"])
        rl = sa4([128, NT, 36], F32)
        for t in range(NT):
            pb = t // 8
            o = (t % 8) * 36
            for c in range(8):
                P.op("pe", lambda e, c=c, t=t, pb=pb, o=o: e.matmul(ps[pb][:, o:o + 36], lhsT=hT[:, c, t * 128:(t + 1) * 128], rhs=wr[:, c, :],
                                                                  start=(c == 0), stop=(c == 7)),
                     reads=[("hT", t // 4), "wr"], writes=[("ps", pb)], inc=(c == 7))
        for pb in range(2):
            P.op("dve", lambda e, pb=pb: e.tensor_tensor(out=rl[:, pb * 8:(pb + 1) * 8, :], in0=ps[pb][:, 0:288].rearrange("p (t n) -> p t n", n=36),
                                                         in1=brB.unsqueeze(1).to_broadcast([128, 8, 36]), op=ALU.add),
                 reads=[("ps", pb), "brB"], writes=["rl" + str(pb)])
        RL = ["rl0", "rl1"]
        gl = rl[:, :, 0:4]
        el = rl[:, :, 4:36].rearrange("p t (g e) -> p t g e", e=8)
        r_gmax = sa4([128, NT], F32)
        r_oh = sa4([128, NT, 4], F32)
        r_ex = sa4([128, NT, 4], F32)
        r_sum = sa4([128, NT], F32)
        r_pg = sa4([128, NT], F32)
        r_em = sa4([128, NT, 4, 8], F32)
        r_es = sa4([128, NT, 8], F32)
        r_m1 = sa4([128, NT], F32)
        r_k1 = sa4([128, NT, 8], F32)
        r_e2 = sa4([128, NT, 8], F32)
        r_m2 = sa4([128, NT], F32)
        r_k2 = sa4([128, NT, 8], F32)
        r_d = sa4([128, NT], F32)
        r_w1 = sa4([128, NT], F32)
        r_w2 = sa4([128, NT], F32)
        r_cs = sa4([128, NT, 8], F32)

        def dv_(fn, reads, writes):
            P.op("dve", fn, reads=reads, writes=writes)

        dv_(lambda e: e.tensor_reduce(out=r_gmax, in_=gl, axis=AX.X, op=ALU.max), RL, ["r_gmax"])
        dv_(lambda e: e.tensor_tensor(out=r_oh, in0=gl, in1=r_gmax.unsqueeze(2).to_broadcast([128, NT, 4]), op=ALU.is_equal),
            RL + ["r_gmax"], ["r_oh"])
        dv_(lambda e: e.tensor_tensor(out=r_ex, in0=gl, in1=r_gmax.unsqueeze(2).to_broadcast([128, NT, 4]), op=ALU.subtract),
            RL + ["r_gmax"], ["r_ex"])
        P.op("act", lambda e: e.activation(out=r_ex, in_=r_ex, func=AF.Exp), reads=["r_ex"], writes=["r_ex"])
        dv_(lambda e: e.tensor_reduce(out=r_sum, in_=r_ex, axis=AX.X, op=ALU.add), ["r_ex"], ["r_sum"])
        dv_(lambda e: e.reciprocal(out=r_pg, in_=r_sum), ["r_sum"], ["r_pg"])
        dv_(lambda e: e.tensor_tensor(out=r_em, in0=el, in1=r_oh.unsqueeze(3).to_broadcast([128, NT, 4, 8]), op=ALU.mult),
            RL + ["r_oh"], ["r_em"])
        dv_(lambda e: e.tensor_reduce(out=r_es, in_=r_em.rearrange("p t g e -> p t e g"), axis=AX.X, op=ALU.add), ["r_em"], ["r_es"])
        dv_(lambda e: e.tensor_reduce(out=r_m1, in_=r_es, axis=AX.X, op=ALU.max), ["r_es"], ["r_m1"])
        dv_(lambda e: e.tensor_tensor(out=r_k1, in0=r_es, in1=r_m1.unsqueeze(2).to_broadcast([128, NT, 8]), op=ALU.is_equal),
            ["r_es", "r_m1"], ["r_k1"])
        dv_(lambda e: e.scalar_tensor_tensor(out=r_e2, in0=r_k1, scalar=-1e30, in1=r_es, op0=ALU.mult, op1=ALU.add),
            ["r_k1", "r_es"], ["r_e2"])
        dv_(lambda e: e.tensor_reduce(out=r_m2, in_=r_e2, axis=AX.X, op=ALU.max), ["r_e2"], ["r_m2"])
        dv_(lambda e: e.tensor_tensor(out=r_k2, in0=r_e2, in1=r_m2.unsqueeze(2).to_broadcast([128, NT, 8]), op=ALU.is_equal),
            ["r_e2", "r_m2"], ["r_k2"])
        dv_(lambda e: e.tensor_tensor(out=r_d, in0=r_m2, in1=r_m1, op=ALU.subtract), ["r_m1", "r_m2"], ["r_d"])
        P.op("act", lambda e: e.activation(out=r_d, in_=r_d, func=AF.Exp), reads=["r_d"], writes=["r_d"])
        dv_(lambda e: e.tensor_scalar(out=r_w1, in0=r_d, scalar1=1.0, scalar2=None, op0=ALU.add), ["r_d"], ["r_w1"])
        dv_(lambda e: e.reciprocal(out=r_w1, in_=r_w1), ["r_w1"], ["r_w1"])
        dv_(lambda e: e.tensor_tensor(out=r_w1, in0=r_w1, in1=r_pg, op=ALU.mult), ["r_w1", "r_pg"], ["r_w1"])
        dv_(lambda e: e.tensor_tensor(out=r_w2, in0=r_w1, in1=r_d, op=ALU.mult), ["r_w1", "r_d"], ["r_w2"])
        dv_(lambda e: e.tensor_tensor(out=r_k1, in0=r_k1, in1=r_w1.unsqueeze(2).to_broadcast([128, NT, 8]), op=ALU.mult),
            ["r_k1", "r_w1"], ["r_k1"])
        dv_(lambda e: e.tensor_tensor(out=r_k2, in0=r_k2, in1=r_w2.unsqueeze(2).to_broadcast([128, NT, 8]), op=ALU.mult),
            ["r_k2", "r_w2"], ["r_k2"])
        dv_(lambda e: e.tensor_tensor(out=r_cs, in0=r_k1, in1=r_k2, op=ALU.add), ["r_k1", "r_k2"], ["r_cs"])
        cw48 = sa4([128, NT, 48], F32)
        P.op("pool", lambda e: e.iota(cw48[:, :, 32:48].bitcast(I32), pattern=[[128, NT], [0, 16]], base=0, channel_multiplier=1),
             writes=["cw_tok"])
        cw4 = cw48[:, :, 0:32].rearrange("p t (g e) -> p t g e", e=8)
        dv_(lambda e: e.tensor_tensor(out=cw4, in0=r_oh.unsqueeze(3).to_broadcast([128, NT, 4, 8]),
                                      in1=r_cs.unsqueeze(2).to_broadcast([128, NT, 4, 8]), op=ALU.mult),
            ["r_oh", "r_cs"], ["cw"])

        chk(4)
        P.scope("p5_moe")
        ohb = sa4([128, NT, 4], BF16)
        ltri = sa4([128, 128], BF16)
        wth = sa4([128, NT, 4], F32)
        csum = sa4([128, NT, 4], F32)
        cum = sa4([128, NT, 4], F32)
        ntot = sa4([128, 4], F32)
        nti = sa4([128, 4], F32).bitcast(I32)
        ntf = sa4([128, 4], F32)
        tsf = sa4([128, 8], F32)
        segs = sa4([128, 4], F32)
        ptmp = sa4([128, NT, 4], F32)
        posf = sa4([128, NT], F32)
        xsb = [sa4([128, D], BF16) for _ in range(2)]
        P.op("pool", lambda e: e.memset(ltri, 1.0), writes=["ltri"])
        P.op("pool", lambda e: e.affine_select(out=ltri, in_=ltri, pattern=[[1, 128]], compare_op=ALU.is_gt, fill=0.0, base=0,
                                               channel_multiplier=-1), reads=["ltri"], writes=["ltri"])
        dv_(lambda e: e.tensor_copy(out=ohb, in_=r_oh), ["r_oh"], ["ohb"])
        oh2 = ohb.rearrange("p t g -> p (t g)")
        P.op("pe", lambda e: e.matmul(ps[2][:, 0:64], lhsT=ltri, rhs=oh2, start=True, stop=True), reads=["ltri", "ohb"], writes=[("ps", 2)])
        P.op("pe", lambda e: e.matmul(ps[3][:, 0:64], lhsT=ones_bf, rhs=oh2, start=True, stop=True), reads=["ones", "ohb"], writes=[("ps", 3)])
        dv_(lambda e: e.tensor_copy(out=wth.rearrange("p t g -> p (t g)"), in_=ps[2][:, 0:64]), [("ps", 2)], ["wth"])
        dv_(lambda e: e.tensor_copy(out=csum.rearrange("p t g -> p (t g)"), in_=ps[3][:, 0:64]), [("ps", 3)], ["csum"])
        dv_(lambda e: e.memset(cum[:, 0, :], 0.0), [], ["cum"])
        for tt in range(1, NT):
            dv_(lambda e, tt=tt: e.tensor_tensor(out=cum[:, tt, :], in0=cum[:, tt - 1, :], in1=csum[:, tt - 1, :], op=ALU.add),
                ["cum", "csum"], ["cum"])
        dv_(lambda e: e.tensor_tensor(out=ntot, in0=cum[:, NT - 1, :], in1=csum[:, NT - 1, :], op=ALU.add), ["cum", "csum"], ["ntot"])
        dv_(lambda e: e.tensor_scalar(out=ntf, in0=ntot, scalar1=127.0, scalar2=None, op0=ALU.add), ["ntot"], ["ntf"])
        dv_(lambda e: e.tensor_copy(out=nti, in_=ntf), ["ntf"], ["nti"])
        dv_(lambda e: e.tensor_single_scalar(out=nti, in_=nti, scalar=7, op=ALU.arith_shift_right), ["nti"], ["nti"])
        dv_(lambda e: e.tensor_copy(out=ntf, in_=nti), ["nti"], ["ntf"])
        dv_(lambda e: e.memset(tsf[:, 0:1], 0.0), [], ["tsf"])
        for g in range(1, 4):
            dv_(lambda e, g=g: e.tensor_tensor(out=tsf[:, g:g + 1], in0=tsf[:, g - 1:g], in1=ntf[:, g - 1:g], op=ALU.add), ["tsf", "ntf"], ["tsf"])
        dv_(lambda e: e.tensor_tensor(out=tsf[:, 4:8], in0=tsf[:, 0:4], in1=ntf, op=ALU.add), ["tsf", "ntf"], ["tsf"])
        dv_(lambda e: e.tensor_copy(out=tsi, in_=tsf), ["tsf"], ["tsi"])
        dv_(lambda e: e.tensor_scalar(out=segs, in0=tsf[:, 0:4], scalar1=128.0, scalar2=None, op0=ALU.mult), ["tsf"], ["segs"])
        dv_(lambda e: e.tensor_tensor(out=ptmp, in0=cum, in1=wth, op=ALU.add), ["cum", "wth"], ["ptmp"])
        dv_(lambda e: e.tensor_tensor(out=ptmp, in0=ptmp, in1=segs.unsqueeze(1).to_broadcast([128, NT, 4]), op=ALU.add), ["ptmp", "segs"], ["ptmp"])
        dv_(lambda e: e.tensor_tensor(out=ptmp, in0=ptmp, in1=r_oh, op=ALU.mult), ["ptmp", "r_oh"], ["ptmp"])
        dv_(lambda e: e.tensor_reduce(out=posf, in_=ptmp, axis=AX.X, op=ALU.add), ["ptmp"], ["posf"])
        dv_(lambda e: e.tensor_copy(out=posi, in_=posf), ["posf"], ["posi"])

        def scat(dst, src, t):
            def f(e):
                try:
                    return e.indirect_dma_start(out=dst, out_offset=bass.IndirectOffsetOnAxis(ap=posi[:, t:t + 1], axis=0),
                                                in_=src, in_offset=None)
                except Exception:
                    print("SCAT FAIL", dst, src, posi[:, t:t + 1], t)
                    raise
            return f

        sc_toks = []
        for t in range(NT):
            xrow = Xr[:, t, :]
            xb = t % 2
            P.op("act", lambda e, xrow=xrow, xb=xb, t=t: e.activation(out=xsb[xb], in_=xrow, func=AF.Identity, scale=rs2[:, t:t + 1],
                                                                     bias=nm2[:, t:t + 1]),
                 reads=[("X", t)] + STAT2[t], writes=[("xsb", xb)])
            sc_toks.append(P.dma("pool", scat(Hn_d[:, :], xsb[xb], t), reads=[("xsb", xb), "posi", "zf_hn"], writes=[("hn_d", t)]))
            P.op("dve", lambda e, xrow=xrow: e.tensor_scalar(out=xrow, in0=xrow, scalar1=ALPHA, scalar2=None, op0=ALU.mult),
                 reads=[("X", t)], writes=[("X", t)])
            sc_toks.append(P.dma("pool", scat(Xs_d[:, :], xrow, t), reads=[("X", t), "posi", "zf_xs"], writes=[("xs_d", t)]))
            sc_toks.append(P.dma("pool", scat(Cs_d[:, :], cw48[:, t, :], t), reads=["cw", "cw_tok", "posi", "zf_cs"], writes=[("cs_d", t)]))

        chk(4)
        P.barrier()
        TB_OFF = 512 + 256 + 256 + 512
        ov = Alloc(TB_OFF, TB_OFF + 6144)
        hidb = ov([128, 4, 128], BF16)
        hidt = [ov([128, 512], BF16) for _ in range(2)]
        sgt5 = ov([128, 512], BF16)
        cws = view(TB_OFF + 6144 + 8192, [128, NSL, 48], F32)
        lnB2 = view(TB_OFF + 6144, [128, 2, D], F32)
        sa5 = Alloc(S_OFF, ARENA_BYTES)
        Wg = [sa5([128, 8, 512], BF16) for _ in range(2)]
        Wu = [sa5([128, 8, 512], BF16) for _ in range(2)]
        Wd = [sa5([128, 4, D], BF16) for _ in range(2)]
        hn4 = Wd[1].rearrange("p a b -> p (a b)")[:, 0:4 * D].rearrange("p (a b) -> p a b", b=D)
        scr6 = [ln_scr(sa5) for _ in range(2)]
        P.dma("sp", lambda e: e.dma_start(out=lnB2[:, 0, :], in_=ln2g_d.partition_broadcast(128)), writes=["ln2B0"])
        P.dma("sp", lambda e: e.dma_start(out=lnB2[:, 1, :], in_=ln2b_d.partition_broadcast(128)), writes=["ln2B1"])
        pre_toks = [P.dma("sp", lambda e: e.dma_start(out=cws, in_=Cs_d.rearrange("(j p) n -> p j n", p=128)), writes=["cws"])]
        for j4 in range(NSL // 4):
            pre_toks.append(P.dma("sp", lambda e, j4=j4: e.dma_start(out=Xr[:, j4 * 4:(j4 + 1) * 4, :],
                                                                     in_=Xs_d[j4 * 512:(j4 + 1) * 512, :].rearrange("(j p) d -> p j d", p=128)),
                                  writes=[("Xs", j4 * 4 + i) for i in range(4)]))
        for j4 in range(NSL // 4):
            P.dma("sp", lambda e, j4=j4: e.dma_start(out=hn4, in_=Hn_d[j4 * 512:(j4 + 1) * 512, :].rearrange("(j p) d -> p j d", p=128)),
                  writes=[("Wd", 1)])
            to_hT(hn4, ("Wd", 1), j4, 3, 2, [4, 5, 6, 7], hT=hTs, hres="hTs")

        def load_expert(ex):
            b = ex % 2
            P.dma("pool", lambda e: e.dma_start(out=Wg[b], in_=wg_d[ex].rearrange("(c p) n -> p c n", p=128)), writes=[("Wg", b)])
            P.dma("pool", lambda e: e.dma_start(out=Wu[b], in_=wu_d[ex].rearrange("(c p) n -> p c n", p=128)), writes=[("Wu", b)])
            P.dma("pool", lambda e: e.dma_start(out=Wd[b], in_=wd_d[ex].rearrange("(c p) n -> p c n", p=128)), writes=[("Wd", b)])
            P.op("pool", lambda e: e.tensor_tensor(out=Wd[b], in0=Wd[b], in1=g2B.unsqueeze(1).to_broadcast([128, 4, D]), op=ALU.mult),
                 reads=[("Wd", b)] + G2B, writes=[("Wd", b)])

        def moe_gu(ex, j):
            b = ex % 2
            par = j % 2
            pg, pu = par, 2 + par
            js = slice(j * 128, (j + 1) * 128)
            hres = ("hTs", j // 4)
            for c in range(8):
                P.op("pe", lambda e, c=c: e.matmul(ps[pg], lhsT=hTs[:, c, js], rhs=Wg[b][:, c, :], start=(c == 0), stop=(c == 7)),
                     reads=[("Wg", b), hres], writes=[("ps", pg)], inc=(c == 7))
            for c in range(8):
                P.op("pe", lambda e, c=c: e.matmul(ps[pu], lhsT=hTs[:, c, js], rhs=Wu[b][:, c, :], start=(c == 0), stop=(c == 7)),
                     reads=[("Wu", b), hres], writes=[("ps", pu)], inc=(c == 7))
            P.op("act", lambda e: e.activation(out=sgt5, in_=ps[pg], func=AF.Silu), reads=[("ps", pg)], writes=["sgt5"])
            P.op("dve", lambda e: e.tensor_tensor(out=hidt[par], in0=ps[pu], in1=sgt5, op=ALU.mult),
                 reads=[("ps", pu), "sgt5"], writes=[("hidt", par)])

        def moe_t(ex, j):
            par = j % 2
            for fc in range(4):
                P.op("pe", lambda e, fc=fc: e.transpose(out=psbf[6][:, fc * 128:(fc + 1) * 128], in_=hidt[par][:, fc * 128:(fc + 1) * 128],
                                                        identity=ident),
                     reads=[("hidt", par), "ident"], writes=[("ps", 6)], inc=(fc == 3))
            P.op("dve", lambda e: e.tensor_copy(out=hidb.rearrange("p a b -> p (a b)"), in_=psbf[6][:, 0:512]),
                 reads=[("ps", 6)], writes=["hidb"])

        def moe_y(ex, j):
            b = ex % 2
            for fc in range(4):
                for hf in range(2):
                    pb = 4 + hf
                    P.op("pe", lambda e, fc=fc, hf=hf, pb=pb: e.matmul(ps[pb], lhsT=hidb[:, fc, :], rhs=Wd[b][:, fc, hf * 512:(hf + 1) * 512],
                                                                      start=(fc == 0), stop=(fc == 3)),
                         reads=["hidb", ("Wd", b)], writes=[("ps", pb)], inc=(fc == 3))
            for hf in range(2):
                pb = 4 + hf
                xs_ = Xr[:, j, hf * 512:(hf + 1) * 512]
                P.op("dve", lambda e, xs_=xs_, pb=pb: e.scalar_tensor_tensor(out=xs_, in0=ps[pb], scalar=cws[:, j, ex:ex + 1], in1=xs_,
                                                                           op0=ALU.mult, op1=ALU.add),
                     reads=[("ps", pb), "cws", ("Xs", j)], writes=[("Xs", j)])

        for i_ in range(2):
            P.op("dve", lambda e, i_=i_: e.memset(hidt[i_], 0.0), writes=[("hidt", i_)])
        for eng_ in ("pe", "act", "dve"):
            P.wait_all(eng_, pre_toks)
        P.op("pe", lambda e: e.matmul(ps[7][0:1, 0:2], lhsT=ident[0:1, 0:1], rhs=ident[0:1, 0:2], start=True, stop=True),
             reads=["ident"], writes=[("ps", 7)])
        for g in range(4):
            P.vload("ts%d" % g, tsi[0:1, g:g + 1], ["tsi"])
            P.vload("te%d" % g, tsi[0:1, 4 + g:5 + g], ["tsi"])
        for ex in range(N_MOE_EXPERTS):
            g = ex // 8
            load_expert(ex)
            asc = g < 2
            TS, TE = "ts%d" % g, "te%d" % g
            NCH = 19
            def c_lo_le(k, asc=asc, TS=TS, TE=TE):
                if asc:
                    return lambda v, k=k: v[TS] <= k
                return lambda v, k=k: v[TE] >= NCH - k
            def c_hi_gt(k, asc=asc, TS=TS, TE=TE):
                if asc:
                    return lambda v, k=k: v[TE] > k
                return lambda v, k=k: v[TS] < NCH - k
            def tile(k, asc=asc):
                return k if asc else NCH - 1 - k
            depth = 0
            for k in range(NCH + 1):
                if k >= 1:
                    P.begin_if(c_hi_gt(k - 1))
                    depth += 1
                P.begin_if(c_lo_le(k) if (asc and g > 0) or not asc else (lambda v, TE=TE: v[TE] >= 0))
                if k >= 1:
                    moe_t(ex, tile(k - 1))
                if k < NCH:
                    P.begin_if(c_hi_gt(k))
                    moe_gu(ex, tile(k))
                    P.end_if()
                if k >= 1:
                    moe_y(ex, tile(k - 1))
                P.end_if()
            for _ in range(depth):
                P.end_if()
        chk(5)
        stopped = False
    except _Stop:
        P.barrier()
        stopped = True

    P.scope("p6_ln2_out")
    if stopped:
        otoks = []
        for t in range(NT):
            otoks.append(P.dma("sp", lambda e, t=t: e.dma_start(out=y_d[t * 128:(t + 1) * 128, :], in_=Xr[:, t, :]), reads=[("X", t)]))
    else:
        stoks = []
        otoks = []
        scr6l = [ln_scr(sa5) for _ in range(4)]
        for j4 in range(NSL // 4):
            js_ = [j4 * 4 + i for i in range(4) if j4 * 4 + i < 19]
            rrs = {}
            for i, j in enumerate(js_):
                rrs[j] = ln_stats(Xr[:, j, :], ("Xs", j), scr6l[i])
            for i, j in enumerate(js_):
                xrow = Xr[:, j, :]
                scr = scr6l[i]
                P.op("act", lambda e, xrow=xrow, scr=scr: e.activation(out=xrow, in_=xrow, func=AF.Identity, scale=scr[3], bias=scr[4]),
                     reads=[("Xs", j)] + rrs[j], writes=[("Xs", j)])
            for i, j in enumerate(js_):
                xrow = Xr[:, j, :]
                P.op("dve", lambda e, xrow=xrow: e.tensor_tensor(out=xrow, in0=xrow, in1=lnB2[:, 0, :], op=ALU.mult),
                     reads=[("Xs", j), "ln2B0"], writes=[("Xs", j)])
            for i, j in enumerate(js_):
                xrow = Xr[:, j, :]
                P.op("pool", lambda e, xrow=xrow: e.tensor_tensor(out=xrow, in0=xrow, in1=lnB2[:, 1, :], op=ALU.add),
                     reads=[("Xs", j), "ln2B1"], writes=[("Xs", j)])
            for j in js_:
                otoks.append(P.dma("pool", lambda e, j=j: e.indirect_dma_start(
                    out=y_d[:, :], out_offset=bass.IndirectOffsetOnAxis(ap=cws[:, j, 32:33].bitcast(I32), axis=0),
                    in_=Xr[:, j, :], in_offset=None, bounds_check=S - 1, oob_is_err=False),
                    reads=[("Xs", j), "cws"]))
    P.wait_all("sp", otoks)
    P.emit()
    es.close()
    return nc


def _host_tables(rel_bias):
    rel = (np.arange(128)[:, None] - np.arange(384)[None, :] + 128).astype(np.int32)
    half, max_exact, max_dist = 16, 8, 128
    ret = np.where(rel > 0, half, 0)
    n = np.abs(rel)
    nf = np.maximum(n, 1).astype(np.float32)
    lg = np.log(nf / np.float32(max_exact)).astype(np.float32) / np.float32(math.log(max_dist / max_exact)) * np.float32(half - max_exact)
    large = max_exact + lg.astype(np.float32).astype(np.int32)
    large = np.minimum(large, half - 1)
    bucket = ret + np.where(n < max_exact, n, large)
    rb = np.asarray(rel_bias, dtype=np.float32)
    tb = rb[bucket]
    tb = np.ascontiguousarray(tb.transpose(0, 2, 1)).reshape(128, 4 * 384)
    cb = np.zeros((128, 8), np.float32)
    for h in range(4):
        cb[:, 2 * h + 0] = rb[15, h]
        cb[:, 2 * h + 1] = rb[31, h]
    tpos = np.arange(S)
    row = (tpos // 64).astype(np.float32)
    col = (tpos % 64).astype(np.float32)
    freqs = (np.float32(10000.0) ** (-np.arange(0, 32, 2, dtype=np.float32) / np.float32(32))).astype(np.float32)
    ang = np.concatenate([row[:, None] * freqs, col[:, None] * freqs], -1).astype(np.float32)
    cos = np.cos(ang).astype(np.float32)
    sin = np.sin(ang).astype(np.float32)
    C = np.repeat(cos, 2, axis=1)
    Sg = np.stack([-sin, sin], -1).reshape(S, 64)
    C = np.ascontiguousarray(C.reshape(NT, 128, 64).transpose(1, 0, 2)).reshape(128, NT * 64)
    Sg = np.ascontiguousarray(Sg.reshape(NT, 128, 64).transpose(1, 0, 2)).reshape(128, NT * 64)
    return tb.astype(np.float32), cb, C.astype(np.float32), Sg.astype(np.float32)


_CACHE = {}


def kernel(x, c, w_ada, b_ada, w_in, lambda_q1, lambda_k1, lambda_q2, lambda_k2, diff_subln_g, q_norm_g, k_norm_g,
           rel_bias, w_out, ln1_g, ln1_b, w_router_group, b_router_group, w_router_expert, b_router_expert,
           w_gate, w_up, w_down, ln2_g, ln2_b):
    f = lambda a: np.ascontiguousarray(np.asarray(a, dtype=np.float32))
    x = f(x)
    c = f(c)
    tb, cb, rc, rs = _host_tables(rel_bias)
    w_rt = np.concatenate([f(w_router_group)[0], f(w_router_expert)[0].transpose(1, 0, 2).reshape(D, 32)], axis=1)
    b_rt = np.concatenate([f(b_router_group)[0], f(b_router_expert)[0].reshape(32)])
    lam = np.stack([f(lambda_q1)[0], f(lambda_k1)[0], f(lambda_q2)[0], f(lambda_k2)[0]])
    shared = {
        "w_ada": f(w_ada)[0], "b_ada": f(b_ada)[0], "w_in": f(w_in)[0], "lam": f(lam),
        "subg": f(diff_subln_g)[0], "qg": f(q_norm_g)[0], "kg": f(k_norm_g)[0],
        "tbias": tb, "cbias": cb, "ropec": rc, "ropes": rs,
        "w_out": f(w_out)[0], "ln1_g": f(ln1_g)[0], "ln1_b": f(ln1_b)[0],
        "w_rt": f(w_rt), "b_rt": f(b_rt),
        "w_gate": f(w_gate)[0], "w_up": f(w_up)[0], "w_down": f(w_down)[0],
        "ln2_g": f(ln2_g)[0], "ln2_b": f(ln2_b)[0],
    }
    if "nc" not in _CACHE:
        _CACHE["nc"] = build_program()
    nc = _CACHE["nc"]
    in_maps = []
    for b in range(8):
        m = dict(shared)
        m["x"] = x[b]
        m["c"] = c[b]
        in_maps.append(m)
    res = run_bass_kernel_spmd(nc, in_maps, core_ids=list(range(8)))
    out = np.stack([np.asarray(r["y"], dtype=np.float32) for r in res.results], axis=0)
    return out
```
